# Optimizing a Trainium2 kernel written in Bass

```python
import math
import jax, jax.numpy as jnp
from jax import lax
import numpy as np

D_MODEL = 1024
BATCH = 8
SEQ = 4096
DEPTH = 2

HEAD_DIM = 64
A_HEADS = 4
A_VDIM = 2 * HEAD_DIM
B_HEADS = 8
B_BRANCHES = ((128, 1), (512, 4), (2048, 16))
B_BLOCK = 128
C_HEADS = 16
C_Q_RANK = 384
C_KV_RANK = 256
C_NOPE = 64
C_ROPE = 32
C_VDIM = 64
C_SCALE = (C_NOPE + C_ROPE) ** -0.5
IDX_HEADS = 8
IDX_DIM = 64
IDX_ROPE = 32
TOPK_MAX = 256
Q_BLOCK = 128
D_FF = 4 * D_MODEL
PLE_DIM = 256
ROPE_THETA = 10000.0
EPS = 1e-6
N_EVEN = (DEPTH + 1) // 2
N_ODD = DEPTH // 2
AB_SPLITS = (A_HEADS * 2 * HEAD_DIM, A_HEADS * 2 * HEAD_DIM, A_HEADS * A_VDIM,
             B_HEADS * HEAD_DIM, B_HEADS * HEAD_DIM, B_HEADS * HEAD_DIM)
AB_IN = sum(AB_SPLITS)
AB_OUT = A_HEADS * A_VDIM + B_HEADS * HEAD_DIM
C_SPLITS = (C_Q_RANK, C_KV_RANK, C_ROPE, IDX_DIM, IDX_HEADS)
C_IN = sum(C_SPLITS)
C_OUT = C_HEADS * C_VDIM

kernel_name = "hybrid_diff_dilated_dsa_trunk"

F32 = jnp.float32


def rms(x, g):
    xf = x.astype(F32)
    y = xf * lax.rsqrt(jnp.mean(xf * xf, axis=-1, keepdims=True) + EPS) * g.astype(F32)
    return y.astype(x.dtype)


def rope(x, pos):
    half = x.shape[-1] // 2
    inv = ROPE_THETA ** (-jnp.arange(half, dtype=F32) / half)
    ang = pos.astype(F32)[:, None] * inv[None, :]
    cos = jnp.cos(ang)[:, None, :]
    sin = jnp.sin(ang)[:, None, :]
    x1 = x[..., :half].astype(F32)
    x2 = x[..., half:].astype(F32)
    return jnp.concatenate([x1 * cos - x2 * sin, x1 * sin + x2 * cos], axis=-1).astype(x.dtype)


def split_cols(t, sizes):
    offs = np.cumsum(sizes)[:-1].tolist()
    return jnp.split(t, offs, axis=-1)


def diff_attention(q, k, v, lam):
    S = q.shape[3]
    scale = HEAD_DIM ** -0.5
    outs = []
    for start in range(0, S, Q_BLOCK):
        end = start + Q_BLOCK
        s = jnp.einsum('bhmqd,bhmkd->bhmqk', q[:, :, :, start:end], k[:, :, :, :end]).astype(F32) * scale
        causal = jnp.arange(start, end)[:, None] >= jnp.arange(end)[None, :]
        pr = jax.nn.softmax(jnp.where(causal, s, -jnp.inf), axis=-1)
        a = pr[:, :, 0] - lam * pr[:, :, 1]
        outs.append(jnp.einsum('bhqk,bhkd->bhqd', a.astype(v.dtype), v[:, :, :end]))
    return jnp.concatenate(outs, axis=2)


def dilated_branch(q, k, v, window, dilation):
    Bn, H, S, dh = q.shape
    steps = window // dilation
    L = S // dilation
    nb = -(-L // B_BLOCK)
    Lp = nb * B_BLOCK

    def strided(t):
        t = t.reshape(Bn, H, L, dilation, dh).transpose(0, 1, 3, 2, 4)
        t = jnp.pad(t, ((0, 0), (0, 0), (0, 0), (0, Lp - L), (0, 0)))
        return t.reshape(Bn, H, dilation, nb, B_BLOCK, dh)

    def with_prev(t):
        prev = jnp.pad(t, ((0, 0), (0, 0), (0, 0), (1, 0), (0, 0), (0, 0)))[:, :, :, :-1]
        return jnp.concatenate([prev, t], axis=-2)

    qb = strided(q)
    kk = with_prev(strided(k))
    vv = with_prev(strided(v))
    s = jnp.einsum('bhcnqd,bhcnkd->bhcnqk', qb, kk).astype(F32) * dh ** -0.5
    ki = jnp.arange(2 * B_BLOCK)[None, :]
    dist = (jnp.arange(B_BLOCK)[:, None] + B_BLOCK) - ki
    blk = jnp.arange(nb)[:, None, None]
    ok = (dist >= 0) & (dist <= steps) & ((blk > 0) | (ki >= B_BLOCK))
    s = jnp.where(ok, s, -jnp.inf)
    lse = jax.nn.logsumexp(s, axis=-1)
    pr = jnp.exp(s - lse[..., None])
    o = jnp.einsum('bhcnqk,bhcnkd->bhcnqd', pr.astype(v.dtype), vv)
    o = o.reshape(Bn, H, dilation, Lp, dh)[:, :, :, :L].transpose(0, 1, 3, 2, 4).reshape(Bn, H, S, dh)
    lse = lse.reshape(Bn, H, dilation, Lp)[..., :L].transpose(0, 1, 3, 2).reshape(Bn, H, S)
    return o, lse


def dilated_attention(q, k, v):
    res = [dilated_branch(q, k, v, w, d) for (w, d) in B_BRANCHES]
    outs = jnp.stack([r[0] for r in res]).astype(F32)
    lses = jnp.stack([r[1] for r in res])
    wts = jax.nn.softmax(lses, axis=0)
    return jnp.einsum('gbhs,gbhsd->bhsd', wts, outs)


def mixer_ab(h, pos, w_in, w_out, lq1, lk1, lq2, lk2, g_sub, lam_init):
    Bn, S, _ = h.shape
    aq, ak, av, bq, bk, bv = split_cols(h @ w_in, AB_SPLITS)
    def a_qk(t):
        t = rope(t.reshape(Bn, S, A_HEADS * 2, HEAD_DIM), pos)
        return t.reshape(Bn, S, A_HEADS, 2, HEAD_DIM).transpose(0, 2, 3, 1, 4)
    av = av.reshape(Bn, S, A_HEADS, A_VDIM).transpose(0, 2, 1, 3)
    lam = (jnp.exp(jnp.sum(lq1.astype(F32) * lk1.astype(F32)))
           - jnp.exp(jnp.sum(lq2.astype(F32) * lk2.astype(F32))) + lam_init)
    ao = diff_attention(a_qk(aq), a_qk(ak), av, lam)
    ao = rms(ao, g_sub) * (1.0 - lam_init)
    ao = ao.transpose(0, 2, 1, 3).reshape(Bn, S, A_HEADS * A_VDIM)
    def b_heads(t, rot):
        t = t.reshape(Bn, S, B_HEADS, HEAD_DIM)
        t = rope(t, pos) if rot else t
        return t.transpose(0, 2, 1, 3)
    bo = dilated_attention(b_heads(bq, True), b_heads(bk, True), b_heads(bv, False))
    bo = bo.transpose(0, 2, 1, 3).reshape(Bn, S, B_HEADS * HEAD_DIM).astype(h.dtype)
    return jnp.concatenate([ao.astype(h.dtype), bo], axis=-1) @ w_out


def partial_rope(t, pos):
    return jnp.concatenate([rope(t[..., :IDX_ROPE], pos), t[..., IDX_ROPE:]], axis=-1)


def mixer_c(h, pos, w_in, g_cq, g_ckv, w_uq, w_qi, w_uk, w_uv, w_out):
    Bn, S, _ = h.shape
    cq, ckv, krope, kidx, widx = split_cols(h @ w_in, C_SPLITS)
    cq = rms(cq, g_cq)
    ckv = rms(ckv, g_ckv)
    q = (cq @ w_uq).reshape(Bn, S, C_HEADS, C_NOPE + C_ROPE)
    q_lat = jnp.einsum('bshd,chd->bshc', q[..., :C_NOPE], w_uk)
    q_full = jnp.concatenate([q_lat, rope(q[..., C_NOPE:], pos)], axis=-1)
    k_rope = rope(krope[:, :, None, :], pos)[:, :, 0]
    kv = jnp.concatenate([ckv, k_rope], axis=-1)
    qi = partial_rope((cq @ w_qi).reshape(Bn, S, IDX_HEADS, IDX_DIM), pos)
    ki = partial_rope(kidx[:, :, None, :], pos)[:, :, 0]
    wi = widx.astype(F32) * IDX_HEADS ** -0.5
    k_sel = min(TOPK_MAX, S // 4)
    nb = S // Q_BLOCK

    def blocks(t):
        return t.reshape((Bn, nb, Q_BLOCK) + t.shape[2:]).swapaxes(0, 1)

    def one_block(args):
        qf, qib, wib, start = args
        tq = start + jnp.arange(Q_BLOCK)
        dots = jnp.einsum('bqhd,bsd->bqhs', qib, ki).astype(F32)
        score = jnp.einsum('bqh,bqhs->bqs', wib, jax.nn.relu(dots)) * IDX_DIM ** -0.5
        causal = jnp.arange(S)[None, :] <= tq[:, None]
        score = jnp.where(causal, score, -jnp.inf)
        _, idx = lax.top_k(score, k_sel)
        valid = idx <= tq[None, :, None]
        sel = jax.vmap(lambda kvb, ib: kvb[ib])(kv, idx)
        s = jnp.einsum('bqhc,bqkc->bqhk', qf, sel).astype(F32) * C_SCALE
        pr = jax.nn.softmax(jnp.where(valid[:, :, None, :], s, -jnp.inf), axis=-1)
        o_lat = jnp.einsum('bqhk,bqkc->bqhc', pr.astype(sel.dtype), sel[..., :C_KV_RANK])
        return jnp.einsum('bqhc,chd->bqhd', o_lat, w_uv)

    o = lax.map(one_block, (blocks(q_full), blocks(qi), blocks(wi), jnp.arange(nb) * Q_BLOCK))
    o = o.swapaxes(0, 1).reshape(Bn, S, C_OUT)
    return o @ w_out


def setup_inputs(seed: int = 0) -> dict:
    key = jax.random.key(seed)
    ks = iter(jax.random.split(key, 32))

    def nrm(shape, fan_in):
        return jax.random.normal(next(ks), shape, F32) * fan_in ** -0.5

    def gain(shape):
        return 1.0 + 0.02 * jax.random.normal(next(ks), shape, F32)

    def small(shape):
        return 0.1 * jax.random.normal(next(ks), shape, F32)

    return {
        "x": jax.random.normal(next(ks), (BATCH, SEQ, D_MODEL), F32),
        "p": jax.random.normal(next(ks), (DEPTH, BATCH, SEQ, PLE_DIM), F32),
        "g_mix_pre": gain((DEPTH, D_MODEL)),
        "g_mix_post": gain((DEPTH, D_MODEL)),
        "g_mlp_pre": gain((DEPTH, D_MODEL)),
        "g_mlp_post": gain((DEPTH, D_MODEL)),
        "w_mlp_in": nrm((DEPTH, D_MODEL, D_FF), D_MODEL),
        "w_mlp_out": nrm((DEPTH, D_FF, D_MODEL), D_FF),
        "w_ple_proj": nrm((DEPTH, PLE_DIM, D_MODEL), PLE_DIM),
        "w_ple_gate": nrm((DEPTH, D_MODEL, D_MODEL), D_MODEL),
        "w_in_ab": nrm((N_EVEN, D_MODEL, AB_IN), D_MODEL),
        "w_out_ab": nrm((N_EVEN, AB_OUT, D_MODEL), AB_OUT),
        "diff_lq1": small((N_EVEN, HEAD_DIM)),
        "diff_lk1": small((N_EVEN, HEAD_DIM)),
        "diff_lq2": small((N_EVEN, HEAD_DIM)),
        "diff_lk2": small((N_EVEN, HEAD_DIM)),
        "g_diff_sub": gain((N_EVEN, A_VDIM)),
        "w_in_c": nrm((N_ODD, D_MODEL, C_IN), D_MODEL),
        "g_cq": gain((N_ODD, C_Q_RANK)),
        "g_ckv": gain((N_ODD, C_KV_RANK)),
        "w_uq": nrm((N_ODD, C_Q_RANK, C_HEADS * (C_NOPE + C_ROPE)), C_Q_RANK),
        "w_qi": nrm((N_ODD, C_Q_RANK, IDX_HEADS * IDX_DIM), C_Q_RANK),
        "w_uk": nrm((N_ODD, C_KV_RANK, C_HEADS, C_NOPE), C_KV_RANK),
        "w_uv": nrm((N_ODD, C_KV_RANK, C_HEADS, C_VDIM), C_KV_RANK),
        "w_out_c": nrm((N_ODD, C_OUT, D_MODEL), C_OUT),
    }


def reference(x, p, g_mix_pre, g_mix_post, g_mlp_pre, g_mlp_post, w_mlp_in, w_mlp_out,
              w_ple_proj, w_ple_gate, w_in_ab, w_out_ab, diff_lq1, diff_lk1, diff_lq2,
              diff_lk2, g_diff_sub, w_in_c, g_cq, g_ckv, w_uq, w_qi, w_uk, w_uv, w_out_c):
    S = x.shape[1]
    pos = jnp.arange(S)
    h = x
    for i in range(DEPTH):
        hn = rms(h, g_mix_pre[i])
        j = i // 2
        if i % 2 == 0:
            lam_init = 0.8 - 0.6 * math.exp(-0.3 * i)
            y = mixer_ab(hn, pos, w_in_ab[j], w_out_ab[j], diff_lq1[j], diff_lk1[j],
                         diff_lq2[j], diff_lk2[j], g_diff_sub[j], lam_init)
        else:
            y = mixer_c(hn, pos, w_in_c[j], g_cq[j], g_ckv[j], w_uq[j], w_qi[j],
                        w_uk[j], w_uv[j], w_out_c[j])
        h = h + rms(y, g_mix_post[i])
        hn = rms(h, g_mlp_pre[i])
        y = jnp.square(jax.nn.relu(hn @ w_mlp_in[i])) @ w_mlp_out[i]
        h = h + rms(y, g_mlp_post[i])
        gate = jax.nn.sigmoid(h @ w_ple_gate[i])
        h = h + gate * (p[i] @ w_ple_proj[i])
    return h
```

```python
import numpy as np
from contextlib import ExitStack
import concourse.bass as bass
import concourse.mybir as mybir
from concourse.bass_utils import run_bass_kernel_spmd

F32 = mybir.dt.float32
BF16 = mybir.dt.bfloat16
ALU = mybir.AluOpType
ACT = mybir.ActivationFunctionType

S = 4096
NT = S // 128
D = 1024
EPS = 1e-6
NEG = -1.0e30
C_SCALE = 96.0 ** -0.5
LIM_H = 99
LIM_Q = NT
B0_STOP = 4
LIM_T = NT
A1_STOP = 9
B0_M = 2


class Buf:
    def __init__(self, name, ap=None, accumulate=False, dsem=None):
        self.name, self.ap, self.accumulate, self.dsem = name, ap, accumulate, dsem
        self.writers = {}
        self.readers = {}

    def __getitem__(self, k):
        return self.ap[k]


class E:
    def __init__(self, name, h, sem):
        self.name, self.h, self.sem = name, h, sem
        self.count = 0
        self.seen = {}


class FW:
    def __init__(self, nc, es, ndsem=56):
        self.nc = nc
        self.engs = {}
        for name, h in (("pe", nc.tensor), ("dve", nc.vector), ("act", nc.scalar),
                        ("pool", nc.gpsimd), ("sp", nc.sync)):
            self.engs[name] = E(name, h, es.enter_context(nc.semaphore("s_" + name)))
        self.pool_sems = [es.enter_context(nc.semaphore("d%d" % i)) for i in range(ndsem)]
        self.semcnt = {}
        self.ps = None
        self.uid = 0

    def begin(self):
        self.ps = ExitStack()
        self.free_sems = list(self.pool_sems)

    def end(self):
        self.barrier()
        self.ps.close()
        self.ps = None

    def sb(self, name, shape, dt=F32, dma=False):
        self.uid += 1
        t = self.ps.enter_context(self.nc.sbuf_tensor("%s_%d" % (name, self.uid), list(shape), dt))
        b = Buf(name, t.ap())
        if dma:
            b.dsem = self.free_sems.pop()
        return b

    def psum(self, name, shape, dt=F32):
        self.uid += 1
        t = self.ps.enter_context(self.nc.psum_tensor("%s_%d" % (name, self.uid), list(shape), dt))
        return Buf(name, t.ap())

    def _wait(self, e, deps):
        for sem, val in deps.items():
            if e.seen.get(sem, 0) >= val:
                continue
            e.h.wait_ge(sem, val)
            e.seen[sem] = val

    def _deps(self, e, reads, writes, is_dma=False):
        deps = {}

        def add(d):
            for s, v in d.items():
                if deps.get(s, 0) < v:
                    deps[s] = v
        for b in reads:
            add(b.writers)
        for b in writes:
            if b.accumulate:
                continue
            add(b.readers)
            if not (is_dma and b.dsem is not None and list(b.writers.keys()) == [b.dsem]):
                add(b.writers)
        if e.name == "pe":
            deps.pop(e.sem, None)
        return deps

    def _commit(self, reads, writes, sem, val):
        for b in reads:
            if b.readers.get(sem, 0) < val:
                b.readers[sem] = val
        for b in writes:
            if b.accumulate:
                if b.writers.get(sem, 0) < val:
                    b.writers[sem] = val
            else:
                if list(b.writers.keys()) == [sem] and not b.readers:
                    b.writers[sem] = val
                else:
                    b.writers = {sem: val}
                b.readers = {}

    def op(self, eng, reads, writes, fn):
        e = self.engs[eng]
        self._wait(e, self._deps(e, reads, writes))
        ins = fn(e.h)
        e.count += 1
        ins.then_inc(e.sem, 1)
        self._commit(reads, writes, e.sem, e.count)
        return ins

    def dma(self, out_buf, out_ap, in_buf, in_ap, q="sp"):
        e = self.engs[q]
        sbb = out_buf if out_buf.dsem is not None else in_buf
        assert sbb.dsem is not None, (out_buf.name, in_buf.name)
        self._wait(e, self._deps(e, [in_buf], [out_buf], is_dma=True))
        ins = e.h.dma_start(out=out_ap, in_=in_ap)
        cnt = self.semcnt.get(sbb.dsem, 0) + 16
        self.semcnt[sbb.dsem] = cnt
        ins.then_inc(sbb.dsem, 16)
        self._commit([in_buf], [out_buf], sbb.dsem, cnt)
        return ins

    def barrier(self):
        deps = dict(self.semcnt)
        for e in self.engs.values():
            if e.count:
                deps[e.sem] = e.count
        for e in self.engs.values():
            d = dict(deps)
            self._wait(e, d)


class Ring:
    def __init__(self, bufs):
        self.bufs, self.i = bufs, -1

    def next(self):
        self.i = (self.i + 1) % len(self.bufs)
        return self.bufs[self.i]


def bc_mid(ap2d, n):
    p, f = ap2d.shape
    return ap2d.unsqueeze(1).to_broadcast([p, n, f])


def bc_last(ap2d, n):
    p, f = ap2d.shape
    return ap2d.unsqueeze(2).to_broadcast([p, f, n])


def rstd_from_ss(fw, ss, n):
    fw.op("act", [ss], [ss], lambda h: h.activation(out=ss[:, 0:1], in_=ss[:, 0:1], func=ACT.Sqrt,
                                                    scale=1.0 / n, bias=EPS))
    fw.op("dve", [ss], [ss], lambda h: h.reciprocal(out=ss[:, 0:1], in_=ss[:, 0:1]))


def rms(fw, srcb, src, n, gb, g, dstb, dst, ss, junk):
    fw.op("act", [srcb], [junk, ss], lambda h: h.activation(out=junk[:, 0:n], in_=src, func=ACT.Square,
                                                            accum_out=ss[:, 0:1]))
    rstd_from_ss(fw, ss, n)
    fw.op("dve", [srcb, ss, gb], [dstb], lambda h: h.scalar_tensor_tensor(
        out=dst, in0=src, scalar=ss[:, 0:1], in1=g, op0=ALU.mult, op1=ALU.mult))


def transposes(fw, srcb, src_aps, psb, ps_aps, ident, identb):
    for s_ap, p_ap in zip(src_aps, ps_aps):
        fw.op("pe", [srcb, identb], [psb], lambda h, s_ap=s_ap, p_ap=p_ap: h.transpose(
            out=p_ap, in_=s_ap, identity=ident))


def load_bcast(fw, dst, dram_buf, row_ap, n):
    fw.dma(dst, dst[:, 0:n], dram_buf, row_ap.partition_broadcast(128))


def rope_emit(fw, srcb, src3, dstb, dst3, tabb, cos2, sin2, nh, half, tmpb, tmp3a, tmp3b):
    cb, sb_ = bc_mid(cos2, nh), bc_mid(sin2, nh)
    x1, x2 = src3[:, :, 0:half], src3[:, :, half:2 * half]
    d1, d2 = dst3[:, :, 0:half], dst3[:, :, half:2 * half]
    fw.op("dve", [srcb, tabb], [dstb], lambda h: h.tensor_tensor(out=d1, in0=x1, in1=cb, op=ALU.mult))
    fw.op("dve", [srcb, tabb], [tmpb], lambda h: h.tensor_tensor(out=tmp3a, in0=x2, in1=sb_, op=ALU.mult))
    fw.op("pool", [dstb, tmpb], [dstb], lambda h: h.tensor_tensor(out=d1, in0=d1, in1=tmp3a, op=ALU.subtract))
    fw.op("dve", [srcb, tabb], [dstb], lambda h: h.tensor_tensor(out=d2, in0=x2, in1=cb, op=ALU.mult))
    fw.op("dve", [srcb, tabb], [tmpb], lambda h: h.tensor_tensor(out=tmp3b, in0=x1, in1=sb_, op=ALU.mult))
    fw.op("pool", [dstb, tmpb], [dstb], lambda h: h.tensor_tensor(out=d2, in0=d2, in1=tmp3b, op=ALU.add))


def phase_A0(fw, X, W, G, C):
    fw.begin()
    w = fw.sb("w", [128, 8, 3072], dma=True)
    for k in range(8):
        fw.dma(w, w[:, k, :], W["w_in_ab"], W["w_in_ab"].ap[k * 128:(k + 1) * 128, :])
    g = fw.sb("g", [128, D], dma=True)
    load_bcast(fw, g, W["g_mix_pre"], W["g_mix_pre"].ap[0:1, :], D)
    ident = fw.sb("ident", [128, 128], dma=True)
    fw.dma(ident, ident[:, :], C["ident"], C["ident"].ap)
    xts = Ring([fw.sb("xt%d" % i, [128, D], dma=True) for i in range(2)])
    tabs = Ring([fw.sb("tab%d" % i, [128, 64], dma=True) for i in range(2)])
    hn = fw.sb("hn", [128, D])
    hnT = fw.sb("hnT", [128, 8, 128])
    junk = fw.sb("junk", [128, D])
    ss = fw.sb("ss", [128, 1])
    qk = fw.sb("qk", [128, 4, 8, 64])
    tmp = fw.sb("tmp", [128, 2, 8, 32])
    vts = Ring([fw.sb("vt%d" % i, [128, D], dma=True) for i in range(2)])
    qkTs = Ring([fw.sb("qkT%d" % i, [128, 16, 128], dma=True) for i in range(2)])
    pT = fw.psum("pT", [128, 1024])
    pm = Ring([fw.psum("pm%d" % i, [128, 512]) for i in range(2)])
    pq = fw.psum("pq", [128, 2048])
    for t in range(min(NT, LIM_T)):
        r = slice(t * 128, (t + 1) * 128)
        xt = xts.next()
        tab = tabs.next()
        fw.dma(xt, xt[:, :], X, X.ap[r, :])
        fw.dma(tab, tab[:, :], C["tab64"], C["tab64"].ap[r, :])
        rms(fw, xt, xt[:, :], D, g, g[:, :], hn, hn[:, :], ss, junk)
        transposes(fw, hn, [hn[:, k * 128:(k + 1) * 128] for k in range(8)], pT,
                   [pT[:, k * 128:(k + 1) * 128] for k in range(8)], ident[:, :], ident)
        fw.op("act", [pT], [hnT], lambda h: h.copy(out=hnT[:, :, :], in_=pT[:, :].rearrange("p (k t) -> p k t", k=8)))
        vt = vts.next()
        qi = 0
        for c in range(6):
            p = pm.next()
            for k in range(8):
                fw.op("pe", [hnT, w], [p], lambda h, k=k, c=c, p=p: h.matmul(
                    out=p[:, :], lhsT=hnT[:, k, :], rhs=w[:, k, c * 512:(c + 1) * 512], start=(k == 0), stop=(k == 7)))
            if c in (2, 5):
                vo = 0 if c == 2 else 512
                fw.op("act", [p], [vt], lambda h, p=p, vo=vo: h.copy(out=vt[:, vo:vo + 512], in_=p[:, :]))
            else:
                src3 = p[:, :].rearrange("p (h d) -> p h d", h=8)
                rope_emit(fw, p, src3, qk, qk[:, qi, :, :], tab, tab[:, 0:32], tab[:, 32:64], 8, 32,
                          tmp, tmp[:, 0, :, :], tmp[:, 1, :, :])
                qi += 1
        fw.dma(W["v0"], W["v0"].ap[r, :], vt, vt[:, :])
        qkf = qk[:, :, :, :].rearrange("p a h d -> p (a h d)")
        transposes(fw, qk, [qkf[:, j * 128:(j + 1) * 128] for j in range(16)], pq,
                   [pq[:, j * 128:(j + 1) * 128] for j in range(16)], ident[:, :], ident)
        qkT = qkTs.next()
        fw.op("act", [pq], [qkT], lambda h, qkT=qkT: h.copy(out=qkT[:, 0:8, :], in_=pq[:, 0:1024].rearrange("p (k t) -> p k t", k=8)))
        fw.op("dve", [pq], [qkT], lambda h, qkT=qkT: h.tensor_copy(out=qkT[:, 8:16, :], in_=pq[:, 1024:2048].rearrange("p (k t) -> p k t", k=8)))
        fw.dma(W["qkT0"], W["qkT0"].ap[:, :, r].rearrange("c p s -> p c s"), qkT, qkT[:, :, :])
    fw.end()


def attn_epilogue_store(fw, dst_dram, dst_ap, ot):
    fw.dma(dst_dram, dst_ap, ot, ot[:, :])


def phase_B0(fw, W, C):
    fw.begin()
    cm = fw.sb("cm", [128, 128], dma=True)
    fw.dma(cm, cm[:, :], C["cmaskT"], C["cmaskT"].ap)
    lv = fw.sb("lv", [128, 4, 64], dma=True)
    for i, nm in enumerate(("diff_lq1", "diff_lk1", "diff_lq2", "diff_lk2")):
        fw.dma(lv, lv[:, i, :], W[nm], W[nm].ap[0:1, :].partition_broadcast(128))
    gs = fw.sb("gs", [128, 128], dma=True)
    load_bcast(fw, gs, W["g_diff_sub"], W["g_diff_sub"].ap[0:1, :], 128)
    lt = fw.sb("lt", [128, 2, 64])
    ls = fw.sb("ls", [128, 4])
    fw.op("dve", [lv], [lt], lambda h: h.tensor_tensor(out=lt[:, 0, :], in0=lv[:, 0, :], in1=lv[:, 1, :], op=ALU.mult))
    fw.op("dve", [lv], [lt], lambda h: h.tensor_tensor(out=lt[:, 1, :], in0=lv[:, 2, :], in1=lv[:, 3, :], op=ALU.mult))
    fw.op("dve", [lt], [ls], lambda h: h.reduce_sum(out=ls[:, 0:2], in_=lt[:, :, :], axis=mybir.AxisListType.X))
    fw.op("act", [ls], [ls], lambda h: h.activation(out=ls[:, 0:2], in_=ls[:, 0:2], func=ACT.Exp))
    fw.op("dve", [ls], [ls], lambda h: h.tensor_tensor(out=ls[:, 2:3], in0=ls[:, 1:2], in1=ls[:, 0:1], op=ALU.subtract))
    fw.op("dve", [ls], [ls], lambda h: h.tensor_scalar(out=ls[:, 2:3], in0=ls[:, 2:3], scalar1=-0.2, scalar2=None, op0=ALU.add))
    fw.op("dve", [gs], [gs], lambda h: h.tensor_scalar(out=gs[:, :], in0=gs[:, :], scalar1=0.8, scalar2=None, op0=ALU.mult))
    qTs = Ring([fw.sb("qT%d" % i, [128, S], dma=True) for i in range(2)])
    kTs = Ring([fw.sb("kT%d" % i, [128, 2, S], dma=True) for i in range(2)])
    for kT in kTs.bufs:
        fw.op("pool", [], [kT], lambda h, kT=kT: h.memset(kT[64:128, 0, :], 0.0))
        fw.op("pool", [], [kT], lambda h, kT=kT: h.memset(kT[0:64, 1, :], 0.0))
    vas = Ring([fw.sb("va%d" % i, [128, NT, 130], dma=True) for i in range(2)])
    for va in vas.bufs:
        fw.op("pool", [], [va], lambda h, va=va: h.memset(va[:, :, :], 1.0))
    Ps = Ring([fw.sb("P%d" % i, [128, 256]) for i in range(3)])
    pss = Ring([fw.psum("ps%d" % i, [128, 512]) for i in range(3)])
    accs = Ring([fw.psum("acc%d" % i, [128, 512]) for i in range(2)])
    rr = fw.sb("rr", [128, 4])
    o1 = fw.sb("o1", [128, 128])
    junk = fw.sb("junk", [128, 128])
    ss = fw.sb("ss", [128, 1])
    ots = Ring([fw.sb("ot%d" % i, [128, 128], dma=True) for i in range(2)])
    for hd in range(min(4, LIM_H) if B0_STOP >= 2 else 0):
        qT, kT, va = qTs.next(), kTs.next(), vas.next()
        fw.dma(qT, qT[:, :], W["qkT0"], W["qkT0"].ap[hd, :, :])
        fw.dma(kT, kT[0:64, 0, :], W["qkT0"], W["qkT0"].ap[4 + hd, 0:64, :])
        fw.dma(kT, kT[64:128, 1, :], W["qkT0"], W["qkT0"].ap[4 + hd, 64:128, :])
        for n0 in range(0, NT, 8):
            fw.dma(va, va[:, n0:n0 + 8, 0:128], W["v0"], W["v0"].ap[n0 * 128:(n0 + 8) * 128, hd * 128:(hd + 1) * 128].rearrange("(n p) d -> p n d", p=128))
        for qi in range(LIM_Q):
            acc = accs.next()
            qs = slice(qi * 128, (qi + 1) * 128)
            for kb in range(qi + 1):
                ks = slice(kb * 128, (kb + 1) * 128)
                ps, P = pss.next(), Ps.next()
                for m in range(B0_M):
                    fw.op("pe", [qT, kT], [ps], lambda h, m=m, ps=ps, ks=ks: h.matmul(
                        out=ps[:, m * 128:(m + 1) * 128], lhsT=kT[:, m, ks], rhs=qT[:, qs], start=True, stop=True))
                fw.op("act", [ps], [P], lambda h, ps=ps, P=P: h.activation(out=P[:, :], in_=ps[:, 0:256], func=ACT.Exp, scale=0.125))
                if kb == qi:
                    fw.op("dve", [P, cm], [P], lambda h, P=P: h.tensor_tensor(
                        out=P[:, :].rearrange("p (m q) -> p m q", m=2), in0=P[:, :].rearrange("p (m q) -> p m q", m=2),
                        in1=bc_mid(cm[:, :], 2), op=ALU.mult))
                for m in range(2 if B0_STOP >= 3 else 0):
                    fw.op("pe", [P, va], [acc], lambda h, m=m, P=P, kb=kb, acc=acc: h.matmul(
                        out=acc[:, m * 130:(m + 1) * 130], lhsT=P[:, m * 128:(m + 1) * 128], rhs=va[:, kb, :],
                        start=(kb == 0 and m == 0), stop=(kb == qi), skip_group_check=True))
            if B0_STOP < 4:
                continue
            a3 = acc[:, 0:260].rearrange("p (m d) -> p m d", m=2)
            fw.op("dve", [acc], [rr], lambda h, a3=a3: h.reciprocal(out=rr[:, 0:2], in_=a3[:, :, 128]))
            fw.op("dve", [rr, ls], [rr], lambda h: h.tensor_tensor(out=rr[:, 2:3], in0=rr[:, 1:2], in1=ls[:, 2:3], op=ALU.mult))
            fw.op("dve", [acc, rr], [o1], lambda h, a3=a3: h.tensor_scalar(out=o1[:, :], in0=a3[:, 0, 0:128], scalar1=rr[:, 0:1], scalar2=None, op0=ALU.mult))
            fw.op("dve", [acc, rr, o1], [o1], lambda h, a3=a3: h.scalar_tensor_tensor(
                out=o1[:, :], in0=a3[:, 1, 0:128], scalar=rr[:, 2:3], in1=o1[:, :], op0=ALU.mult, op1=ALU.add))
            ot = ots.next()
            rms(fw, o1, o1[:, :], 128, gs, gs[:, :], ot, ot[:, :], ss, junk)
            fw.dma(W["attn0"], W["attn0"].ap[qs, hd * 128:(hd + 1) * 128], ot, ot[:, :])
    fw.end()


def phase_C0(fw, W, C):
    fw.begin()
    dm = fw.sb("dm", [128, 17, 128], dma=True)
    for r0, r1 in ((0, 8), (8, 17)):
        fw.dma(dm, dm[:, r0:r1, :], C["dmaskT"], C["dmaskT"].ap[r0:r1].rearrange("r k q -> k r q"))
    qTs = Ring([fw.sb("qT%d" % i, [128, 2, S], dma=True) for i in range(1)])
    kTs = Ring([fw.sb("kT%d" % i, [128, 4, S], dma=True) for i in range(1)])
    for kT in kTs.bufs:
        for j in range(4):
            zr = slice(64, 128) if j % 2 == 0 else slice(0, 64)
            fw.op("pool", [], [kT], lambda h, kT=kT, j=j, zr=zr: h.memset(kT[zr, j, :], 0.0))
    vas = Ring([fw.sb("va%d" % i, [128, NT, 4, 66], dma=True) for i in range(1)])
    for va in vas.bufs:
        fw.op("pool", [], [va], lambda h, va=va: h.memset(va[:, :, :, :], 1.0))
    Ps = Ring([fw.sb("P%d" % i, [128, 512]) for i in range(3)])
    pss = Ring([fw.psum("ps%d" % i, [128, 512]) for i in range(3)])
    accs = Ring([fw.psum("acc%d" % i, [128, 512]) for i in range(2)])
    rr = fw.sb("rr", [128, 4])
    ots = Ring([fw.sb("ot%d" % i, [128, 4, 64], dma=True) for i in range(2)])
    flip = 0
    for g in range(min(2, LIM_H)):
        qT, kT, va = qTs.next(), kTs.next(), vas.next()
        for j in range(2):
            fw.dma(qT, qT[:, j, :], W["qkT0"], W["qkT0"].ap[8 + 2 * g + j, :, :])
            fw.dma(kT, kT[0:64, 2 * j, :], W["qkT0"], W["qkT0"].ap[12 + 2 * g + j, 0:64, :])
            fw.dma(kT, kT[64:128, 2 * j + 1, :], W["qkT0"], W["qkT0"].ap[12 + 2 * g + j, 64:128, :])
        for j in range(4):
            c0 = 512 + (4 * g + j) * 64
            for n0 in range(0, NT, 8):
                fw.dma(va, va[:, n0:n0 + 8, j, 0:64], W["v0"], W["v0"].ap[n0 * 128:(n0 + 8) * 128, c0:c0 + 64].rearrange("(n p) d -> p n d", p=128))
        for qi in range(LIM_Q):
            acc = accs.next()
            qs = slice(qi * 128, (qi + 1) * 128)
            kb0 = max(0, qi - 16)
            for kb in range(kb0, qi + 1):
                ks = slice(kb * 128, (kb + 1) * 128)
                ps, P = pss.next(), Ps.next()
                for j in range(4):
                    fw.op("pe", [qT, kT], [ps], lambda h, j=j, ps=ps, ks=ks: h.matmul(
                        out=ps[:, j * 128:(j + 1) * 128], lhsT=kT[:, j, ks], rhs=qT[:, j // 2, qs], start=True, stop=True))
                fw.op("act", [ps], [P], lambda h, ps=ps, P=P: h.activation(out=P[:, :], in_=ps[:, :], func=ACT.Exp, scale=0.125))
                eng = "dve" if flip else "pool"
                flip ^= 1
                fw.op(eng, [P, dm], [P], lambda h, P=P, kb=kb: h.tensor_tensor(
                    out=P[:, :].rearrange("p (m q) -> p m q", m=4), in0=P[:, :].rearrange("p (m q) -> p m q", m=4),
                    in1=bc_mid(dm[:, qi - kb, :], 4), op=ALU.mult))
                for j in range(4):
                    fw.op("pe", [P, va], [acc], lambda h, j=j, P=P, kb=kb, acc=acc: h.matmul(
                        out=acc[:, j * 66:(j + 1) * 66], lhsT=P[:, j * 128:(j + 1) * 128], rhs=va[:, kb, j, :],
                        start=(kb == kb0 and j == 0), stop=(kb == qi), skip_group_check=True))
            a3 = acc[:, 0:264].rearrange("p (m d) -> p m d", m=4)
            fw.op("dve", [acc], [rr], lambda h, a3=a3: h.reciprocal(out=rr[:, 0:4], in_=a3[:, :, 64]))
            ot = ots.next()
            fw.op("dve", [acc, rr], [ot], lambda h, a3=a3, ot=ot: h.tensor_tensor(
                out=ot[:, :, :], in0=a3[:, :, 0:64], in1=bc_last(rr[:, 0:4], 64), op=ALU.mult))
            c0 = 512 + g * 256
            fw.dma(W["attn0"], W["attn0"].ap[qs, c0:c0 + 256], ot, ot[:, :, :].rearrange("p m d -> p (m d)"))
    fw.end()


def phase_D(fw, W, C, attn, wname, li, hin, hout):
    fw.begin()
    w = fw.sb("w", [128, 8, D], dma=True)
    fw.dma(w, w[:, :, :], W[wname], W[wname].ap.rearrange("(k p) n -> p k n", p=128))
    g = fw.sb("g", [128, D], dma=True)
    load_bcast(fw, g, W["g_mix_post"], W["g_mix_post"].ap[li:li + 1, :], D)
    ident = fw.sb("ident", [128, 128], dma=True)
    fw.dma(ident, ident[:, :], C["ident"], C["ident"].ap)
    ats = Ring([fw.sb("at%d" % i, [128, D], dma=True) for i in range(2)])
    hts = Ring([fw.sb("ht%d" % i, [128, D], dma=True) for i in range(2)])
    aT = fw.sb("aT", [128, 8, 128])
    yn = fw.sb("yn", [128, D])
    junk = fw.sb("junk", [128, D])
    ss = fw.sb("ss", [128, 1])
    pT = fw.psum("pT", [128, 1024])
    py = fw.psum("py", [128, 1024])
    for t in range(min(NT, LIM_T)):
        r = slice(t * 128, (t + 1) * 128)
        at, ht = ats.next(), hts.next()
        fw.dma(at, at[:, :], attn, attn.ap[r, :])
        fw.dma(ht, ht[:, :], hin, hin.ap[r, :])
        transposes(fw, at, [at[:, k * 128:(k + 1) * 128] for k in range(8)], pT,
                   [pT[:, k * 128:(k + 1) * 128] for k in range(8)], ident[:, :], ident)
        fw.op("act", [pT], [aT], lambda h: h.copy(out=aT[:, :, :], in_=pT[:, :].rearrange("p (k t) -> p k t", k=8)))
        for c in range(2):
            for k in range(8):
                fw.op("pe", [aT, w], [py], lambda h, k=k, c=c: h.matmul(
                    out=py[:, c * 512:(c + 1) * 512], lhsT=aT[:, k, :], rhs=w[:, k, c * 512:(c + 1) * 512],
                    start=(k == 0), stop=(k == 7)))
        rms(fw, py, py[:, :], D, g, g[:, :], yn, yn[:, :], ss, junk)
        fw.op("pool", [ht, yn], [ht], lambda h, ht=ht: h.tensor_tensor(out=ht[:, :], in0=ht[:, :], in1=yn[:, :], op=ALU.add))
        fw.dma(hout, hout.ap[r, :], ht, ht[:, :])
    fw.end()


def phase_E(fw, W, C, li, hin, hout):
    fw.begin()
    win = fw.sb("win", [128, 8, 4096], BF16)
    wout = fw.sb("wout", [128, 32, D], BF16)
    stg = Ring([fw.sb("stg%d" % i, [128, 2048], dma=True) for i in range(2)])
    n = 0
    for k in range(8):
        for c in range(2):
            s = stg.next()
            fw.dma(s, s[:, :], W["w_mlp_in"], W["w_mlp_in"].ap[li, k * 128:(k + 1) * 128, c * 2048:(c + 1) * 2048])
            fw.op(("dve", "act")[n % 2], [s], [win], (lambda h, s=s, k=k, c=c: h.tensor_copy(out=win[:, k, c * 2048:(c + 1) * 2048], in_=s[:, :])) if n % 2 == 0 else (lambda h, s=s, k=k, c=c: h.copy(out=win[:, k, c * 2048:(c + 1) * 2048], in_=s[:, :])))
            n += 1
    for k2 in range(16):
        s = stg.next()
        fw.dma(s, s[:, :].rearrange("p (a n) -> p a n", a=2), W["w_mlp_out"],
               W["w_mlp_out"].ap[li, k2 * 256:(k2 + 1) * 256, :].rearrange("(a p) n -> p a n", p=128))
        fw.op(("dve", "act")[n % 2], [s], [wout], (lambda h, s=s, k2=k2: h.tensor_copy(
            out=wout[:, 2 * k2:2 * k2 + 2, :], in_=s[:, :].rearrange("p (a n) -> p a n", a=2))) if n % 2 == 0 else (lambda h, s=s, k2=k2: h.copy(
            out=wout[:, 2 * k2:2 * k2 + 2, :], in_=s[:, :].rearrange("p (a n) -> p a n", a=2))))
        n += 1
    gpre = fw.sb("gpre", [128, D], dma=True)
    load_bcast(fw, gpre, W["g_mlp_pre"], W["g_mlp_pre"].ap[li:li + 1, :], D)
    gpost = fw.sb("gpost", [128, D], dma=True)
    load_bcast(fw, gpost, W["g_mlp_post"], W["g_mlp_post"].ap[li:li + 1, :], D)
    ident = fw.sb("ident", [128, 128], dma=True)
    fw.dma(ident, ident[:, :], C["ident"], C["ident"].ap)
    hts = Ring([fw.sb("ht%d" % i, [128, D], dma=True) for i in range(2)])
    hn = fw.sb("hn", [128, D])
    hnT = fw.sb("hnT", [128, 8, 128], BF16)
    hid = fw.sb("hid", [128, 32, 128], BF16)
    rl = Ring([fw.sb("rl%d" % i, [128, 512]) for i in range(2)])
    yn = fw.sb("yn", [128, D])
    junk = fw.sb("junk", [128, D])
    ss = fw.sb("ss", [128, 1])
    pT = fw.psum("pT", [128, 1024])
    pus = Ring([fw.psum("pu%d" % i, [128, 512]) for i in range(3)])
    py = fw.psum("py", [128, 1024])
    for t in range(min(NT, LIM_T)):
        r = slice(t * 128, (t + 1) * 128)
        ht = hts.next()
        fw.dma(ht, ht[:, :], hin, hin.ap[r, :])
        rms(fw, ht, ht[:, :], D, gpre, gpre[:, :], hn, hn[:, :], ss, junk)
        transposes(fw, hn, [hn[:, k * 128:(k + 1) * 128] for k in range(8)], pT,
                   [pT[:, k * 128:(k + 1) * 128] for k in range(8)], ident[:, :], ident)
        fw.op("act", [pT], [hnT], lambda h: h.copy(out=hnT[:, :, :], in_=pT[:, :].rearrange("p (k t) -> p k t", k=8)))
        for f4 in range(8):
            pu = pus.next()
            for f in range(4):
                fc = f4 * 4 + f
                for k in range(8):
                    fw.op("pe", [hnT, win], [pu], lambda h, k=k, f=f, fc=fc, pu=pu: h.matmul(
                        out=pu[:, f * 128:(f + 1) * 128], lhsT=win[:, k, fc * 128:(fc + 1) * 128], rhs=hnT[:, k, :],
                        start=(k == 0), stop=(k == 7)))
            rt = rl.next()
            fw.op("act", [pu], [rt], lambda h, pu=pu, rt=rt: h.activation(out=rt[:, :], in_=pu[:, :], func=ACT.Relu))
            fw.op("dve", [rt], [hid], lambda h, rt=rt, f4=f4: h.tensor_tensor(
                out=hid[:, f4 * 4:(f4 + 1) * 4, :].rearrange("p a t -> p (a t)"), in0=rt[:, :], in1=rt[:, :], op=ALU.mult))
        for c in range(2):
            for k in range(32):
                fw.op("pe", [hid, wout], [py], lambda h, k=k, c=c: h.matmul(
                    out=py[:, c * 512:(c + 1) * 512], lhsT=hid[:, k, :], rhs=wout[:, k, c * 512:(c + 1) * 512],
                    start=(k == 0), stop=(k == 31)))
        rms(fw, py, py[:, :], D, gpost, gpost[:, :], yn, yn[:, :], ss, junk)
        fw.op("pool", [ht, yn], [ht], lambda h, ht=ht: h.tensor_tensor(out=ht[:, :], in0=ht[:, :], in1=yn[:, :], op=ALU.add))
        fw.dma(hout, hout.ap[r, :], ht, ht[:, :])
    fw.end()


def phase_F(fw, W, C, P_, li, hin, hout):
    fw.begin()
    wg = fw.sb("wg", [128, 8, D], dma=True)
    fw.dma(wg, wg[:, :, :], W["w_ple_gate"], W["w_ple_gate"].ap[li].rearrange("(k p) n -> p k n", p=128))
    wp = fw.sb("wp", [128, 2, D], dma=True)
    fw.dma(wp, wp[:, :, :], W["w_ple_proj"], W["w_ple_proj"].ap[li].rearrange("(k p) n -> p k n", p=128))
    ident = fw.sb("ident", [128, 128], dma=True)
    fw.dma(ident, ident[:, :], C["ident"], C["ident"].ap)
    hts = Ring([fw.sb("ht%d" % i, [128, D], dma=True) for i in range(2)])
    pts = Ring([fw.sb("pt%d" % i, [128, 256], dma=True) for i in range(2)])
    hT = fw.sb("hT", [128, 10, 128])
    gate = fw.sb("gate", [128, D])
    pT = fw.psum("pT", [128, 1536])
    pg = fw.psum("pg", [128, 1024])
    pp = fw.psum("pp", [128, 1024])
    for t in range(min(NT, LIM_T)):
        r = slice(t * 128, (t + 1) * 128)
        ht, pt = hts.next(), pts.next()
        fw.dma(ht, ht[:, :], hin, hin.ap[r, :])
        fw.dma(pt, pt[:, :], P_, P_.ap[li, r, :])
        transposes(fw, ht, [ht[:, k * 128:(k + 1) * 128] for k in range(8)], pT,
                   [pT[:, k * 128:(k + 1) * 128] for k in range(8)], ident[:, :], ident)
        transposes(fw, pt, [pt[:, k * 128:(k + 1) * 128] for k in range(2)], pT,
                   [pT[:, (8 + k) * 128:(9 + k) * 128] for k in range(2)], ident[:, :], ident)
        fw.op("act", [pT], [hT], lambda h: h.copy(out=hT[:, :, :], in_=pT[:, 0:1280].rearrange("p (k t) -> p k t", k=10)))
        for c in range(2):
            for k in range(8):
                fw.op("pe", [hT, wg], [pg], lambda h, k=k, c=c: h.matmul(
                    out=pg[:, c * 512:(c + 1) * 512], lhsT=hT[:, k, :], rhs=wg[:, k, c * 512:(c + 1) * 512],
                    start=(k == 0), stop=(k == 7)))
        for c in range(2):
            for k in range(2):
                fw.op("pe", [hT, wp], [pp], lambda h, k=k, c=c: h.matmul(
                    out=pp[:, c * 512:(c + 1) * 512], lhsT=hT[:, 8 + k, :], rhs=wp[:, k, c * 512:(c + 1) * 512],
                    start=(k == 0), stop=(k == 1)))
        fw.op("act", [pg], [gate], lambda h: h.activation(out=gate[:, :], in_=pg[:, :], func=ACT.Sigmoid))
        fw.op("dve", [pp, gate], [gate], lambda h: h.tensor_tensor(out=gate[:, :], in0=pp[:, :], in1=gate[:, :], op=ALU.mult))
        fw.op("pool", [ht, gate], [ht], lambda h, ht=ht: h.tensor_tensor(out=ht[:, :], in0=ht[:, :], in1=gate[:, :], op=ALU.add))
        fw.dma(hout, hout.ap[r, :], ht, ht[:, :])
    fw.end()


def phase_A1(fw, W, C, hin):
    fw.begin()
    wc = fw.sb("wc", [128, 8, 744], dma=True)
    fw.dma(wc, wc[:, :, :], W["w_in_c"], W["w_in_c"].ap.rearrange("(k p) n -> p k n", p=128))
    wuq = fw.sb("wuq", [128, 3, 1536], dma=True)
    fw.dma(wuq, wuq[:, :, :], W["w_uq"], W["w_uq"].ap.rearrange("(k p) n -> p k n", p=128))
    wqi = fw.sb("wqi", [128, 3, 512], dma=True)
    fw.dma(wqi, wqi[:, :, :], W["w_qi"], W["w_qi"].ap.rearrange("(k p) n -> p k n", p=128))
    wuk = fw.sb("wuk", [128, 2, 1024], dma=True)
    fw.dma(wuk, wuk[:, :, :], W["w_uk"], W["w_uk"].ap.rearrange("(k p) n -> p k n", p=128))
    wuv = fw.sb("wuv", [128, 2, 1024], dma=True)
    fw.dma(wuv, wuv[:, :, :], W["w_uv"], W["w_uv"].ap.rearrange("(k p) n -> p k n", p=128))
    g = fw.sb("g", [128, D], dma=True)
    load_bcast(fw, g, W["g_mix_pre"], W["g_mix_pre"].ap[1:2, :], D)
    gq = fw.sb("gq", [128, 384], dma=True)
    load_bcast(fw, gq, W["g_cq"], W["g_cq"].ap[0:1, :], 384)
    gkv = fw.sb("gkv", [128, 256], dma=True)
    load_bcast(fw, gkv, W["g_ckv"], W["g_ckv"].ap[0:1, :], 256)
    ident = fw.sb("ident", [128, 128], dma=True)
    fw.dma(ident, ident[:, :], C["ident"], C["ident"].ap)
    pA = fw.psum("pA", [128, 2048])
    pB = fw.psum("pB", [128, 2048])
    wukT = fw.sb("wukT", [64, 16, 256])
    for cc in range(2):
        transposes(fw, wuk, [wuk[:, cc, hh * 64:(hh + 1) * 64] for hh in range(16)], pA,
                   [pA[0:64, hh * 128:(hh + 1) * 128] for hh in range(16)], ident[:, :], ident)
        fw.op("act", [pA], [wukT], lambda h, cc=cc: h.copy(
            out=wukT[:, :, cc * 128:(cc + 1) * 128], in_=pA[0:64, :].rearrange("p (h c) -> p h c", h=16)))
    hts = Ring([fw.sb("ht%d" % i, [128, D], dma=True) for i in range(2)])
    tabs = Ring([fw.sb("tab%d" % i, [128, 32], dma=True) for i in range(2)])
    hn = fw.sb("hn", [128, D])
    hnT = fw.sb("hnT", [128, 8, 128])
    junk = fw.sb("junk", [128, D])
    ss = fw.sb("ss", [128, 1])
    csb = fw.sb("csb", [128, 744])
    cqn = fw.sb("cqn", [128, 384])
    cqT = fw.sb("cqT", [128, 3, 128])
    kvtok = fw.sb("kvtok", [128, 288])
    kis = fw.sb("kis", [128, 64])
    tmp = fw.sb("tmp", [128, 2, 16, 16])
    wis = Ring([fw.sb("wis%d" % i, [128, 8], dma=True) for i in range(2)])
    qn = fw.sb("qn", [128, 16, 64])
    qsb = fw.sb("qsb", [128, 1536])
    qr = fw.sb("qr", [128, 16, 32])
    qnT = fw.sb("qnT", [64, 16, 128])
    qfTs = Ring([fw.sb("qfT%d" % i, [128, 3, 2048], dma=True) for i in range(1)])
    kvTs = Ring([fw.sb("kvT%d" % i, [128, 3, 128], dma=True) for i in range(2)])
    vhs = Ring([fw.sb("vh%d" % i, [128, D], dma=True) for i in range(2)])
    qis = fw.sb("qis", [128, 8, 64])
    qiTs = Ring([fw.sb("qiT%d" % i, [64, 8, 128], dma=True) for i in range(2)])
    kiTs = Ring([fw.sb("kiT%d" % i, [64, 128], dma=True) for i in range(2)])
    for t in range(min(NT, LIM_T) if A1_STOP >= 2 else 0):
        r = slice(t * 128, (t + 1) * 128)
        ht, tab = hts.next(), tabs.next()
        fw.dma(ht, ht[:, :], hin, hin.ap[r, :])
        fw.dma(tab, tab[:, :], C["tab32"], C["tab32"].ap[r, :])
        cos, sin = tab[:, 0:16], tab[:, 16:32]
        rms(fw, ht, ht[:, :], D, g, g[:, :], hn, hn[:, :], ss, junk)
        transposes(fw, hn, [hn[:, k * 128:(k + 1) * 128] for k in range(8)], pA,
                   [pA[:, k * 128:(k + 1) * 128] for k in range(8)], ident[:, :], ident)
        fw.op("act", [pA], [hnT], lambda h: h.copy(out=hnT[:, :, :], in_=pA[:, 0:1024].rearrange("p (k t) -> p k t", k=8)))
        for c, (c0, cw) in enumerate(((0, 512), (512, 232))):
            for k in range(8):
                fw.op("pe", [hnT, wc], [pB], lambda h, k=k, c=c, c0=c0, cw=cw: h.matmul(
                    out=pB[:, c * 512:c * 512 + cw], lhsT=hnT[:, k, :], rhs=wc[:, k, c0:c0 + cw], start=(k == 0), stop=(k == 7)))
        fw.op("act", [pB], [csb], lambda h: h.copy(out=csb[:, 0:512], in_=pB[:, 0:512]))
        fw.op("dve", [pB], [csb], lambda h: h.tensor_copy(out=csb[:, 512:744], in_=pB[:, 512:744]))
        rms(fw, csb, csb[:, 0:384], 384, gq, gq[:, :], cqn, cqn[:, :], ss, junk)
        rms(fw, csb, csb[:, 384:640], 256, gkv, gkv[:, :], kvtok, kvtok[:, 0:256], ss, junk)
        rope_emit(fw, csb, csb[:, 640:672].rearrange("p (h d) -> p h d", h=1), kvtok,
                  kvtok[:, 256:288].rearrange("p (h d) -> p h d", h=1), tab, cos, sin, 1, 16,
                  tmp, tmp[:, 0, 0:1, :], tmp[:, 1, 0:1, :])
        rope_emit(fw, csb, csb[:, 672:704].rearrange("p (h d) -> p h d", h=1), kis,
                  kis[:, 0:32].rearrange("p (h d) -> p h d", h=1), tab, cos, sin, 1, 16,
                  tmp, tmp[:, 0, 0:1, :], tmp[:, 1, 0:1, :])
        fw.op("act", [csb], [kis], lambda h: h.copy(out=kis[:, 32:64], in_=csb[:, 704:736]))
        wi = wis.next()
        fw.op("act", [csb], [wi], lambda h, wi=wi: h.mul(out=wi[:, :], in_=csb[:, 736:744], mul=float(8.0 ** -0.5 * 64.0 ** -0.5)))
        fw.dma(W["wi"], W["wi"].ap[r, :], wi, wi[:, :])
        if A1_STOP < 3:
            continue
        transposes(fw, cqn, [cqn[:, k * 128:(k + 1) * 128] for k in range(3)], pA,
                   [pA[:, 1024 + k * 128:1024 + (k + 1) * 128] for k in range(3)], ident[:, :], ident)
        fw.op("act", [pA], [cqT], lambda h: h.copy(out=cqT[:, :, :], in_=pA[:, 1024:1408].rearrange("p (k t) -> p k t", k=3)))
        for c in range(3):
            for k in range(3):
                fw.op("pe", [cqT, wuq], [pB], lambda h, k=k, c=c: h.matmul(
                    out=pB[:, c * 512:(c + 1) * 512], lhsT=cqT[:, k, :], rhs=wuq[:, k, c * 512:(c + 1) * 512],
                    start=(k == 0), stop=(k == 2)))
        fw.op("act", [pB], [qsb], lambda h: h.copy(out=qsb[:, 0:1024], in_=pB[:, 0:1024]))
        fw.op("dve", [pB], [qsb], lambda h: h.tensor_copy(out=qsb[:, 1024:1536], in_=pB[:, 1024:1536]))
        q3 = qsb[:, :].rearrange("p (h d) -> p h d", h=16)
        fw.op("act", [qsb], [qn], lambda h, q3=q3: h.copy(out=qn[:, :, :], in_=q3[:, :, 0:64]))
        rope_emit(fw, qsb, q3[:, :, 64:96], qr, qr[:, :, :], tab, cos, sin, 16, 16, tmp, tmp[:, 0, :, :], tmp[:, 1, :, :])
        for k in range(3):
            fw.op("pe", [cqT, wqi], [pB], lambda h, k=k: h.matmul(
                out=pB[:, 1536:2048], lhsT=cqT[:, k, :], rhs=wqi[:, k, :], start=(k == 0), stop=(k == 2)))
        qi3 = pB[:, 1536:2048].rearrange("p (h d) -> p h d", h=8)
        rope_emit(fw, pB, qi3[:, :, 0:32], qis, qis[:, :, 0:32], tab, cos, sin, 8, 16, tmp, tmp[:, 0, 0:8, :], tmp[:, 1, 0:8, :])
        fw.op("act", [pB], [qis], lambda h, qi3=qi3: h.copy(out=qis[:, :, 32:64], in_=qi3[:, :, 32:64]))
        if A1_STOP < 4:
            continue
        transposes(fw, qn, [qn[:, hh, :] for hh in range(16)], pA,
                   [pA[0:64, hh * 128:(hh + 1) * 128] for hh in range(16)], ident[:, :], ident)
        fw.op("act", [pA], [qnT], lambda h: h.copy(out=qnT[:, :, :], in_=pA[0:64, :].rearrange("p (h t) -> p h t", h=16)))
        qfT = qfTs.next()
        for cc in range(2):
            for hh in range(16):
                fw.op("pe", [qnT, wukT], [pA], lambda h, hh=hh, cc=cc: h.matmul(
                    out=pA[:, hh * 128:(hh + 1) * 128], lhsT=wukT[:, hh, cc * 128:(cc + 1) * 128], rhs=qnT[:, hh, :],
                    start=True, stop=True))
            fw.op(("act", "dve")[cc], [pA], [qfT], (lambda h, qfT=qfT: h.copy(out=qfT[:, 0, :], in_=pA[:, :])) if cc == 0 else
                  (lambda h, qfT=qfT: h.tensor_copy(out=qfT[:, 1, :], in_=pA[:, :])))
        transposes(fw, qr, [qr[:, hh, :] for hh in range(16)], pA,
                   [pA[0:32, hh * 128:(hh + 1) * 128] for hh in range(16)], ident[:, :], ident)
        fw.op("act", [pA], [qfT], lambda h, qfT=qfT: h.copy(out=qfT[0:32, 2, :], in_=pA[0:32, :]))
        fw.dma(W["qfT"], W["qfT"].ap[t, 0:256, :].rearrange("(c p) n -> p c n", p=128), qfT, qfT[:, 0:2, :])
        fw.dma(W["qfT"], W["qfT"].ap[t, 256:288, :], qfT, qfT[0:32, 2, :])
        if A1_STOP < 5:
            continue
        kvT = kvTs.next()
        transposes(fw, kvtok, [kvtok[:, 0:128], kvtok[:, 128:256]], pA,
                   [pA[:, 0:128], pA[:, 128:256]], ident[:, :], ident)
        transposes(fw, kvtok, [kvtok[:, 256:288]], pA, [pA[0:32, 256:384]], ident[:, :], ident)
        fw.op("act", [pA], [kvT], lambda h, kvT=kvT: h.copy(out=kvT[:, 0:2, :], in_=pA[:, 0:256].rearrange("p (k t) -> p k t", k=2)))
        fw.op("dve", [pA], [kvT], lambda h, kvT=kvT: h.tensor_copy(out=kvT[0:32, 2, :], in_=pA[0:32, 256:384]))
        fw.dma(W["kvT"], W["kvT"].ap[0:256, r].rearrange("(c p) s -> p c s", p=128), kvT, kvT[:, 0:2, :])
        fw.dma(W["kvT"], W["kvT"].ap[256:288, r], kvT, kvT[0:32, 2, :])
        for c in range(2):
            for k in range(2):
                fw.op("pe", [kvT, wuv], [pB], lambda h, k=k, c=c, kvT=kvT: h.matmul(
                    out=pB[:, c * 512:(c + 1) * 512], lhsT=kvT[:, k, :], rhs=wuv[:, k, c * 512:(c + 1) * 512],
                    start=(k == 0), stop=(k == 1)))
        vh = vhs.next()
        fw.op("act", [pB], [vh], lambda h, vh=vh: h.copy(out=vh[:, :], in_=pB[:, 0:1024]))
        fw.dma(W["v1"], W["v1"].ap[r, :], vh, vh[:, :])
        if A1_STOP < 6:
            continue
        transposes(fw, qis, [qis[:, hh, :] for hh in range(8)], pA,
                   [pA[0:64, 512 + hh * 128:512 + (hh + 1) * 128] for hh in range(8)], ident[:, :], ident)
        transposes(fw, kis, [kis[:, :]], pA, [pA[0:64, 1536:1664]], ident[:, :], ident)
        qiT, kiT = qiTs.next(), kiTs.next()
        fw.op("act", [pA], [qiT], lambda h, qiT=qiT: h.copy(out=qiT[:, :, :], in_=pA[0:64, 512:1536].rearrange("p (h t) -> p h t", h=8)))
        fw.op("dve", [pA], [kiT], lambda h, kiT=kiT: h.tensor_copy(out=kiT[:, :], in_=pA[0:64, 1536:1664]))
        fw.dma(W["qiT"], W["qiT"].ap[t, :, :], qiT, qiT[:, :, :].rearrange("p h t -> p (h t)"))
        fw.dma(W["kiT"], W["kiT"].ap[:, r], kiT, kiT[:, :])
    fw.end()


def phase_B1a(fw, W, C):
    fw.begin()
    ident = fw.sb("ident", [128, 128], dma=True)
    fw.dma(ident, ident[:, :], C["ident"], C["ident"].ap)
    cb = fw.sb("cb", [128, 128], dma=True)
    fw.dma(cb, cb[:, :], C["cbias"], C["cbias"].ap)
    kiT = fw.sb("kiT", [64, S], dma=True)
    fw.dma(kiT, kiT[:, :], W["kiT"], W["kiT"].ap)
    qiTs = Ring([fw.sb("qiT%d" % i, [64, 8, 128], dma=True) for i in range(2)])
    wis = Ring([fw.sb("wi%d" % i, [128, 8], dma=True) for i in range(2)])
    score = fw.sb("score", [128, S])
    work = fw.sb("work", [128, S])
    m8 = fw.sb("m8", [128, 8])
    rls = Ring([fw.sb("rl%d" % i, [128, 512]) for i in range(3)])
    mTs = Ring([fw.sb("mT%d" % i, [128, NT, 128], dma=True) for i in range(2)])
    pss = Ring([fw.psum("ps%d" % i, [128, 512]) for i in range(4)])
    pts = Ring([fw.psum("pt%d" % i, [128, 512]) for i in range(2)])
    for qi in range(LIM_Q):
        n = (qi + 1) * 128
        qiT, wi = qiTs.next(), wis.next()
        fw.dma(qiT, qiT[:, :, :].rearrange("p h t -> p (h t)"), W["qiT"], W["qiT"].ap[qi, :, :])
        fw.dma(wi, wi[:, :], W["wi"], W["wi"].ap[qi * 128:(qi + 1) * 128, :])
        for c0 in range(0, n, 512):
            cw = min(512, n - c0)
            for hh in range(8):
                ps, rl = pss.next(), rls.next()
                fw.op("pe", [qiT, kiT], [ps], lambda h, hh=hh, ps=ps, c0=c0, cw=cw, qiT=qiT: h.matmul(
                    out=ps[:, 0:cw], lhsT=qiT[:, hh, :], rhs=kiT[:, c0:c0 + cw], start=True, stop=True))
                fw.op("act", [ps], [rl], lambda h, ps=ps, rl=rl, cw=cw: h.activation(out=rl[:, 0:cw], in_=ps[:, 0:cw], func=ACT.Relu))
                if hh == 0:
                    fw.op("dve", [rl, wi], [score], lambda h, rl=rl, wi=wi, c0=c0, cw=cw: h.tensor_scalar(
                        out=score[:, c0:c0 + cw], in0=rl[:, 0:cw], scalar1=wi[:, 0:1], scalar2=None, op0=ALU.mult))
                else:
                    fw.op("dve", [rl, wi, score], [score], lambda h, rl=rl, wi=wi, c0=c0, cw=cw, hh=hh: h.scalar_tensor_tensor(
                        out=score[:, c0:c0 + cw], in0=rl[:, 0:cw], scalar=wi[:, hh:hh + 1], in1=score[:, c0:c0 + cw],
                        op0=ALU.mult, op1=ALU.add))
        fw.op("dve", [score, cb], [score], lambda h, n=n: h.tensor_tensor(
            out=score[:, n - 128:n], in0=score[:, n - 128:n], in1=cb[:, :], op=ALU.add))
        if qi < 2:
            fw.op("dve", [score], [work], lambda h, n=n: h.tensor_scalar(
                out=work[:, 0:n], in0=score[:, 0:n], scalar1=-1.0e29, scalar2=None, op0=ALU.is_ge))
        else:
            src = score
            for it in range(32):
                fw.op("dve", [src], [m8], lambda h, src=src, n=n: h.max(out=m8[:, :], in_=src[:, 0:n]))
                if it < 31:
                    fw.op("dve", [src, m8], [work], lambda h, src=src, n=n: h.match_replace(
                        out=work[:, 0:n], in_to_replace=m8[:, :], in_values=src[:, 0:n], imm_value=NEG))
                    src = work
            fw.op("dve", [score, m8], [work], lambda h, n=n: h.tensor_scalar(
                out=work[:, 0:n], in0=score[:, 0:n], scalar1=m8[:, 7:8], scalar2=None, op0=ALU.is_ge))
        mT = mTs.next()
        for k0 in range(0, qi + 1, 4):
            kn = min(4, qi + 1 - k0)
            pt = pts.next()
            transposes(fw, work, [work[:, (k0 + j) * 128:(k0 + j + 1) * 128] for j in range(kn)], pt,
                       [pt[:, j * 128:(j + 1) * 128] for j in range(kn)], ident[:, :], ident)
            fw.op("act", [pt], [mT], lambda h, pt=pt, mT=mT, k0=k0, kn=kn: h.copy(
                out=mT[:, k0:k0 + kn, :], in_=pt[:, 0:kn * 128].rearrange("p (k q) -> p k q", k=kn)))
        for k0 in range(0, qi + 1, 8):
            k1 = min(qi + 1, k0 + 8)
            fw.dma(W["maskT"], W["maskT"].ap[qi, :, k0:k1, :], mT, mT[:, k0:k1, :])
    fw.end()


def phase_B1b(fw, W, C):
    fw.begin()
    kvT = fw.sb("kvT", [128, 3, S], dma=True)
    fw.dma(kvT, kvT[:, 0:2, :], W["kvT"], W["kvT"].ap[0:256, :].rearrange("(c p) s -> p c s", p=128))
    fw.dma(kvT, kvT[0:32, 2, :], W["kvT"], W["kvT"].ap[256:288, :])
    qfTs = Ring([fw.sb("qfT%d" % i, [128, 3, 2048], dma=True) for i in range(2)])
    mTs = Ring([fw.sb("mT%d" % i, [128, NT, 128], dma=True) for i in range(2)])
    vas = Ring([fw.sb("va%d" % i, [128, 16, 66], dma=True) for i in range(3)])
    for va in vas.bufs:
        fw.op("pool", [], [va], lambda h, va=va: h.memset(va[:, :, :], 1.0))
    Ps = Ring([fw.sb("P%d" % i, [128, 512]) for i in range(4)])
    pss = Ring([fw.psum("ps%d" % i, [128, 512]) for i in range(4)])
    acc = fw.psum("acc", [128, 2048])
    rr = fw.sb("rr", [128, 16])
    ots = Ring([fw.sb("ot%d" % i, [128, 16, 64], dma=True) for i in range(2)])
    flip = 0
    for qi in range(LIM_Q):
        qs = slice(qi * 128, (qi + 1) * 128)
        qfT, mT = qfTs.next(), mTs.next()
        fw.dma(qfT, qfT[:, 0:2, :], W["qfT"], W["qfT"].ap[qi, 0:256, :].rearrange("(c p) n -> p c n", p=128))
        fw.dma(qfT, qfT[0:32, 2, :], W["qfT"], W["qfT"].ap[qi, 256:288, :])
        for k0 in range(0, qi + 1, 8):
            k1 = min(qi + 1, k0 + 8)
            fw.dma(mT, mT[:, k0:k1, :], W["maskT"], W["maskT"].ap[qi, :, k0:k1, :])
        for kb in range(qi + 1):
            ks = slice(kb * 128, (kb + 1) * 128)
            va = vas.next()
            fw.dma(va, va[:, :, 0:64], W["v1"], W["v1"].ap[ks, :].rearrange("p (h d) -> p h d", h=16))
            for g in range(4):
                ps, P = pss.next(), Ps.next()
                for c in range(3):
                    rows = slice(0, 128) if c < 2 else slice(0, 32)
                    fw.op("pe", [qfT, kvT], [ps], lambda h, c=c, rows=rows, ps=ps, ks=ks, g=g, qfT=qfT: h.matmul(
                        out=ps[:, :], lhsT=kvT[rows, c, ks], rhs=qfT[rows, c, g * 512:(g + 1) * 512],
                        start=(c == 0), stop=(c == 2)))
                fw.op("act", [ps], [P], lambda h, ps=ps, P=P: h.activation(out=P[:, :], in_=ps[:, :], func=ACT.Exp, scale=float(C_SCALE)))
                eng = "dve" if flip else "pool"
                flip ^= 1
                fw.op(eng, [P, mT], [P], lambda h, P=P, kb=kb, mT=mT: h.tensor_tensor(
                    out=P[:, :].rearrange("p (m q) -> p m q", m=4), in0=P[:, :].rearrange("p (m q) -> p m q", m=4),
                    in1=bc_mid(mT[:, kb, :], 4), op=ALU.mult))
                for j in range(4):
                    hh = g * 4 + j
                    bank, slot = hh // 6, hh % 6
                    o0 = bank * 512 + slot * 66
                    fw.op("pe", [P, va], [acc], lambda h, j=j, P=P, va=va, hh=hh, o0=o0, kb=kb, slot=slot: h.matmul(
                        out=acc[:, o0:o0 + 66], lhsT=P[:, j * 128:(j + 1) * 128], rhs=va[:, hh, :],
                        start=(kb == 0 and slot == 0), stop=(kb == qi), skip_group_check=True))
        ot = ots.next()
        for bank in range(3):
            nh = 6 if bank < 2 else 4
            a3 = acc[:, bank * 512:bank * 512 + nh * 66].rearrange("p (m d) -> p m d", m=nh)
            fw.op("dve", [acc], [rr], lambda h, a3=a3, bank=bank, nh=nh: h.reciprocal(out=rr[:, bank * 6:bank * 6 + nh], in_=a3[:, :, 64]))
            fw.op("dve", [acc, rr], [ot], lambda h, a3=a3, ot=ot, bank=bank, nh=nh: h.tensor_tensor(
                out=ot[:, bank * 6:bank * 6 + nh, :], in0=a3[:, :, 0:64], in1=bc_last(rr[:, bank * 6:bank * 6 + nh], 64), op=ALU.mult))
        fw.dma(W["attn1"], W["attn1"].ap[qs, :], ot, ot[:, :, :].rearrange("p m d -> p (m d)"))
    fw.end()


WEIGHT_SHAPES = {
    "g_mix_pre": [2, D], "g_mix_post": [2, D], "g_mlp_pre": [2, D], "g_mlp_post": [2, D],
    "w_mlp_in": [2, D, 4096], "w_mlp_out": [2, 4096, D], "w_ple_proj": [2, 256, D], "w_ple_gate": [2, D, D],
    "w_in_ab": [D, 3072], "w_out_ab": [D, D], "diff_lq1": [1, 64], "diff_lk1": [1, 64], "diff_lq2": [1, 64],
    "diff_lk2": [1, 64], "g_diff_sub": [1, 128], "w_in_c": [D, 744], "g_cq": [1, 384], "g_ckv": [1, 256],
    "w_uq": [384, 1536], "w_qi": [384, 512], "w_uk": [256, 1024], "w_uv": [256, 1024], "w_out_c": [D, D],
}
CONST_SHAPES = {"ident": [128, 128], "tab64": [S, 64], "tab32": [S, 32], "cmaskT": [128, 128],
                "cbias": [128, 128], "dmaskT": [17, 128, 128]}
SCRATCH_SHAPES = {
    "qkT0": [16, 128, S], "v0": [S, D], "attn0": [S, D], "h0a": [S, D], "h0b": [S, D], "h0c": [S, D],
    "qfT": [NT, 288, 2048], "kvT": [288, S], "v1": [S, D], "qiT": [NT, 64, 1024], "kiT": [64, S], "wi": [S, 8],
    "maskT": [NT, 128, NT, 128], "attn1": [S, D], "h1a": [S, D], "h1b": [S, D],
}


def build(only=None, ext_in=(), ext_out=()):
    nc = bass.Bass("TRN2", target_bir_lowering=False)
    W, C = {}, {}
    X = Buf("x", nc.dram_tensor("x", [S, D], F32, kind="ExternalInput").ap(), accumulate=True)
    P_ = Buf("p", nc.dram_tensor("p", [2, S, 256], F32, kind="ExternalInput").ap(), accumulate=True)
    for k, shp in WEIGHT_SHAPES.items():
        W[k] = Buf(k, nc.dram_tensor(k, shp, F32, kind="ExternalInput").ap(), accumulate=True)
    for k, shp in CONST_SHAPES.items():
        C[k] = Buf(k, nc.dram_tensor(k, shp, F32, kind="ExternalInput").ap(), accumulate=True)
    for k, shp in SCRATCH_SHAPES.items():
        kind = "ExternalInput" if k in ext_in else ("ExternalOutput" if k in ext_out else "Internal")
        W[k] = Buf(k, nc.dram_tensor(k, shp, F32, kind=kind).ap(), accumulate=True)
    OUT = Buf("out", nc.dram_tensor("out", [S, D], F32, kind="ExternalOutput").ap(), accumulate=True)
    phases = [
        lambda: phase_A0(fw, X, W, None, C),
        lambda: phase_B0(fw, W, C),
        lambda: phase_C0(fw, W, C),
        lambda: phase_D(fw, W, C, W["attn0"], "w_out_ab", 0, X, W["h0a"]),
        lambda: phase_E(fw, W, C, 0, W["h0a"], W["h0b"]),
        lambda: phase_F(fw, W, C, P_, 0, W["h0b"], W["h0c"]),
        lambda: phase_A1(fw, W, C, W["h0c"]),
        lambda: phase_B1a(fw, W, C),
        lambda: phase_B1b(fw, W, C),
        lambda: phase_D(fw, W, C, W["attn1"], "w_out_c", 1, W["h0c"], W["h1a"]),
        lambda: phase_E(fw, W, C, 1, W["h1a"], W["h1b"]),
        lambda: phase_F(fw, W, C, P_, 1, W["h1b"], OUT),
    ]
    with ExitStack() as es:
        fw = FW(nc, es)
        for i, ph in enumerate(phases):
            if only is not None and i not in only:
                continue
            ph()
        fw.barrier()
    return nc


def make_consts():
    c = {}
    c["ident"] = np.eye(128, dtype=np.float32)
    pos = np.arange(S, dtype=np.float32)
    for half, nm in ((32, "tab64"), (16, "tab32")):
        inv = (np.float32(10000.0) ** (-np.arange(half, dtype=np.float32) / np.float32(half))).astype(np.float32)
        ang = (pos[:, None] * inv[None, :]).astype(np.float32)
        c[nm] = np.concatenate([np.cos(ang), np.sin(ang)], axis=1).astype(np.float32)
    k = np.arange(128)[:, None]
    q = np.arange(128)[None, :]
    c["cmaskT"] = (k <= q).astype(np.float32)
    c["cbias"] = np.where(np.arange(128)[None, :] <= np.arange(128)[:, None], 0.0, NEG).astype(np.float32)
    dm = np.zeros((17, 128, 128), np.float32)
    for rel in range(17):
        dist = rel * 128 + q - k
        for w_, d_ in ((128, 1), (512, 4), (2048, 16)):
            dm[rel] += ((dist >= 0) & (dist <= w_) & (dist % d_ == 0)).astype(np.float32)
    c["dmaskT"] = dm
    return c


def make_in_maps(inputs, ncores=8):
    consts = make_consts()
    shared = {}
    for k in WEIGHT_SHAPES:
        a = np.ascontiguousarray(np.asarray(inputs[k], dtype=np.float32))
        shared[k] = a.reshape(WEIGHT_SHAPES[k])
    shared.update(consts)
    x = np.asarray(inputs["x"], dtype=np.float32)
    p = np.asarray(inputs["p"], dtype=np.float32)
    maps = []
    for b in range(ncores):
        m = dict(shared)
        m["x"] = np.ascontiguousarray(x[b])
        m["p"] = np.ascontiguousarray(p[:, b])
        maps.append(m)
    return maps


def kernel(**inputs):
    nc = build()
    maps = make_in_maps(inputs, 8)
    res = run_bass_kernel_spmd(nc, maps, core_ids=list(range(8)))
    return np.stack([np.asarray(r["out"], dtype=np.float32) for r in res.results], axis=0)
```

```python
import numpy as np
from contextlib import ExitStack
import concourse.bass as bass
import concourse.mybir as mybir
from concourse.bass_utils import run_bass_kernel_spmd

F32 = mybir.dt.float32
BF16 = mybir.dt.bfloat16
ALU = mybir.AluOpType
ACT = mybir.ActivationFunctionType

S = 4096
NT = S // 128
D = 1024
EPS = 1e-6
NEG = -1.0e30
C_SCALE = 96.0 ** -0.5
LIM_H = 99
LIM_Q = NT
B0_STOP = 4
LIM_T = NT
A1_STOP = 9
B0_M = 2


class Buf:
    def __init__(self, name, ap=None, accumulate=False, dsem=None):
        self.name, self.ap, self.accumulate, self.dsem = name, ap, accumulate, dsem
        self.writers = {}
        self.readers = {}

    def __getitem__(self, k):
        return self.ap[k]


class E:
    def __init__(self, name, h, sem):
        self.name, self.h, self.sem = name, h, sem
        self.count = 0
        self.seen = {}


class FW:
    def __init__(self, nc, es, ndsem=56):
        self.nc = nc
        self.engs = {}
        for name, h in (("pe", nc.tensor), ("dve", nc.vector), ("act", nc.scalar),
                        ("pool", nc.gpsimd), ("sp", nc.sync)):
            self.engs[name] = E(name, h, es.enter_context(nc.semaphore("s_" + name)))
        self.pool_sems = [es.enter_context(nc.semaphore("d%d" % i)) for i in range(ndsem)]
        self.semcnt = {}
        self.ps = None
        self.uid = 0

    def begin(self):
        self.ps = ExitStack()
        self.free_sems = list(self.pool_sems)

    def end(self):
        self.barrier()
        self.ps.close()
        self.ps = None

    def sb(self, name, shape, dt=F32, dma=False):
        self.uid += 1
        t = self.ps.enter_context(self.nc.sbuf_tensor("%s_%d" % (name, self.uid), list(shape), dt))
        b = Buf(name, t.ap())
        if dma:
            b.dsem = self.free_sems.pop()
        return b

    def psum(self, name, shape, dt=F32):
        self.uid += 1
        t = self.ps.enter_context(self.nc.psum_tensor("%s_%d" % (name, self.uid), list(shape), dt))
        return Buf(name, t.ap())

    def _wait(self, e, deps):
        for sem, val in deps.items():
            if e.seen.get(sem, 0) >= val:
                continue
            e.h.wait_ge(sem, val)
            e.seen[sem] = val

    def _deps(self, e, reads, writes, is_dma=False):
        deps = {}

        def add(d):
            for s, v in d.items():
                if deps.get(s, 0) < v:
                    deps[s] = v
        for b in reads:
            add(b.writers)
        for b in writes:
            if b.accumulate:
                continue
            add(b.readers)
            if not (is_dma and b.dsem is not None and list(b.writers.keys()) == [b.dsem]):
                add(b.writers)
        if e.name == "pe":
            deps.pop(e.sem, None)
        return deps

    def _commit(self, reads, writes, sem, val):
        for b in reads:
            if b.readers.get(sem, 0) < val:
                b.readers[sem] = val
        for b in writes:
            if b.accumulate:
                if b.writers.get(sem, 0) < val:
                    b.writers[sem] = val
            else:
                if list(b.writers.keys()) == [sem] and not b.readers:
                    b.writers[sem] = val
                else:
                    b.writers = {sem: val}
                b.readers = {}

    def op(self, eng, reads, writes, fn):
        e = self.engs[eng]
        self._wait(e, self._deps(e, reads, writes))
        ins = fn(e.h)
        e.count += 1
        ins.then_inc(e.sem, 1)
        self._commit(reads, writes, e.sem, e.count)
        return ins

    def dma(self, out_buf, out_ap, in_buf, in_ap, q="sp"):
        e = self.engs[q]
        sbb = out_buf if out_buf.dsem is not None else in_buf
        assert sbb.dsem is not None, (out_buf.name, in_buf.name)
        self._wait(e, self._deps(e, [in_buf], [out_buf], is_dma=True))
        ins = e.h.dma_start(out=out_ap, in_=in_ap)
        cnt = self.semcnt.get(sbb.dsem, 0) + 16
        self.semcnt[sbb.dsem] = cnt
        ins.then_inc(sbb.dsem, 16)
        self._commit([in_buf], [out_buf], sbb.dsem, cnt)
        return ins

    def barrier(self):
        deps = dict(self.semcnt)
        for e in self.engs.values():
            if e.count:
                deps[e.sem] = e.count
        for e in self.engs.values():
            d = dict(deps)
            self._wait(e, d)


class Ring:
    def __init__(self, bufs):
        self.bufs, self.i = bufs, -1

    def next(self):
        self.i = (self.i + 1) % len(self.bufs)
        return self.bufs[self.i]


def bc_mid(ap2d, n):
    p, f = ap2d.shape
    return ap2d.unsqueeze(1).to_broadcast([p, n, f])


def bc_last(ap2d, n):
    p, f = ap2d.shape
    return ap2d.unsqueeze(2).to_broadcast([p, f, n])


def rstd_from_ss(fw, ss, n):
    fw.op("act", [ss], [ss], lambda h: h.activation(out=ss[:, 0:1], in_=ss[:, 0:1], func=ACT.Sqrt,
                                                    scale=1.0 / n, bias=EPS))
    fw.op("dve", [ss], [ss], lambda h: h.reciprocal(out=ss[:, 0:1], in_=ss[:, 0:1]))


def rms(fw, srcb, src, n, gb, g, dstb, dst, ss, junk):
    fw.op("act", [srcb], [junk, ss], lambda h: h.activation(out=junk[:, 0:n], in_=src, func=ACT.Square,
                                                            accum_out=ss[:, 0:1]))
    rstd_from_ss(fw, ss, n)
    fw.op("dve", [srcb, ss, gb], [dstb], lambda h: h.scalar_tensor_tensor(
        out=dst, in0=src, scalar=ss[:, 0:1], in1=g, op0=ALU.mult, op1=ALU.mult))


def transposes(fw, srcb, src_aps, psb, ps_aps, ident, identb):
    for s_ap, p_ap in zip(src_aps, ps_aps):
        fw.op("pe", [srcb, identb], [psb], lambda h, s_ap=s_ap, p_ap=p_ap: h.transpose(
            out=p_ap, in_=s_ap, identity=ident))


def load_bcast(fw, dst, dram_buf, row_ap, n):
    fw.dma(dst, dst[:, 0:n], dram_buf, row_ap.partition_broadcast(128))


def rope_emit(fw, srcb, src3, dstb, dst3, tabb, cos2, sin2, nh, half, tmpb, tmp3a, tmp3b):
    cb, sb_ = bc_mid(cos2, nh), bc_mid(sin2, nh)
    x1, x2 = src3[:, :, 0:half], src3[:, :, half:2 * half]
    d1, d2 = dst3[:, :, 0:half], dst3[:, :, half:2 * half]
    fw.op("dve", [srcb, tabb], [dstb], lambda h: h.tensor_tensor(out=d1, in0=x1, in1=cb, op=ALU.mult))
    fw.op("dve", [srcb, tabb], [tmpb], lambda h: h.tensor_tensor(out=tmp3a, in0=x2, in1=sb_, op=ALU.mult))
    fw.op("pool", [dstb, tmpb], [dstb], lambda h: h.tensor_tensor(out=d1, in0=d1, in1=tmp3a, op=ALU.subtract))
    fw.op("dve", [srcb, tabb], [dstb], lambda h: h.tensor_tensor(out=d2, in0=x2, in1=cb, op=ALU.mult))
    fw.op("dve", [srcb, tabb], [tmpb], lambda h: h.tensor_tensor(out=tmp3b, in0=x1, in1=sb_, op=ALU.mult))
    fw.op("pool", [dstb, tmpb], [dstb], lambda h: h.tensor_tensor(out=d2, in0=d2, in1=tmp3b, op=ALU.add))


def phase_A0(fw, X, W, G, C):
    fw.begin()
    w = fw.sb("w", [128, 8, 3072], dma=True)
    for k in range(8):
        fw.dma(w, w[:, k, :], W["w_in_ab"], W["w_in_ab"].ap[k * 128:(k + 1) * 128, :])
    g = fw.sb("g", [128, D], dma=True)
    load_bcast(fw, g, W["g_mix_pre"], W["g_mix_pre"].ap[0:1, :], D)
    ident = fw.sb("ident", [128, 128], dma=True)
    fw.dma(ident, ident[:, :], C["ident"], C["ident"].ap)
    xts = Ring([fw.sb("xt%d" % i, [128, D], dma=True) for i in range(2)])
    tabs = Ring([fw.sb("tab%d" % i, [128, 64], dma=True) for i in range(2)])
    hn = fw.sb("hn", [128, D])
    hnT = fw.sb("hnT", [128, 8, 128])
    junk = fw.sb("junk", [128, D])
    ss = fw.sb("ss", [128, 1])
    qk = fw.sb("qk", [128, 4, 8, 64])
    tmp = fw.sb("tmp", [128, 2, 8, 32])
    vts = Ring([fw.sb("vt%d" % i, [128, D], dma=True) for i in range(2)])
    qkTs = Ring([fw.sb("qkT%d" % i, [128, 16, 128], dma=True) for i in range(2)])
    pT = fw.psum("pT", [128, 1024])
    pm = Ring([fw.psum("pm%d" % i, [128, 512]) for i in range(2)])
    pq = fw.psum("pq", [128, 2048])
    for t in range(min(NT, LIM_T)):
        r = slice(t * 128, (t + 1) * 128)
        xt = xts.next()
        tab = tabs.next()
        fw.dma(xt, xt[:, :], X, X.ap[r, :])
        fw.dma(tab, tab[:, :], C["tab64"], C["tab64"].ap[r, :])
        rms(fw, xt, xt[:, :], D, g, g[:, :], hn, hn[:, :], ss, junk)
        transposes(fw, hn, [hn[:, k * 128:(k + 1) * 128] for k in range(8)], pT,
                   [pT[:, k * 128:(k + 1) * 128] for k in range(8)], ident[:, :], ident)
        fw.op("act", [pT], [hnT], lambda h: h.copy(out=hnT[:, :, :], in_=pT[:, :].rearrange("p (k t) -> p k t", k=8)))
        vt = vts.next()
        qi = 0
        for c in range(6):
            p = pm.next()
            for k in range(8):
                fw.op("pe", [hnT, w], [p], lambda h, k=k, c=c, p=p: h.matmul(
                    out=p[:, :], lhsT=hnT[:, k, :], rhs=w[:, k, c * 512:(c + 1) * 512], start=(k == 0), stop=(k == 7)))
            if c in (2, 5):
                vo = 0 if c == 2 else 512
                fw.op("act", [p], [vt], lambda h, p=p, vo=vo: h.copy(out=vt[:, vo:vo + 512], in_=p[:, :]))
            else:
                src3 = p[:, :].rearrange("p (h d) -> p h d", h=8)
                rope_emit(fw, p, src3, qk, qk[:, qi, :, :], tab, tab[:, 0:32], tab[:, 32:64], 8, 32,
                          tmp, tmp[:, 0, :, :], tmp[:, 1, :, :])
                qi += 1
        fw.dma(W["v0"], W["v0"].ap[r, :], vt, vt[:, :])
        qkf = qk[:, :, :, :].rearrange("p a h d -> p (a h d)")
        transposes(fw, qk, [qkf[:, j * 128:(j + 1) * 128] for j in range(16)], pq,
                   [pq[:, j * 128:(j + 1) * 128] for j in range(16)], ident[:, :], ident)
        qkT = qkTs.next()
        fw.op("act", [pq], [qkT], lambda h, qkT=qkT: h.copy(out=qkT[:, 0:8, :], in_=pq[:, 0:1024].rearrange("p (k t) -> p k t", k=8)))
        fw.op("dve", [pq], [qkT], lambda h, qkT=qkT: h.tensor_copy(out=qkT[:, 8:16, :], in_=pq[:, 1024:2048].rearrange("p (k t) -> p k t", k=8)))
        fw.dma(W["qkT0"], W["qkT0"].ap[:, :, r].rearrange("c p s -> p c s"), qkT, qkT[:, :, :])
    fw.end()


def attn_epilogue_store(fw, dst_dram, dst_ap, ot):
    fw.dma(dst_dram, dst_ap, ot, ot[:, :])


def phase_B0(fw, W, C):
    fw.begin()
    cm = fw.sb("cm", [128, 128], dma=True)
    fw.dma(cm, cm[:, :], C["cmaskT"], C["cmaskT"].ap)
    lv = fw.sb("lv", [128, 4, 64], dma=True)
    for i, nm in enumerate(("diff_lq1", "diff_lk1", "diff_lq2", "diff_lk2")):
        fw.dma(lv, lv[:, i, :], W[nm], W[nm].ap[0:1, :].partition_broadcast(128))
    gs = fw.sb("gs", [128, 128], dma=True)
    load_bcast(fw, gs, W["g_diff_sub"], W["g_diff_sub"].ap[0:1, :], 128)
    lt = fw.sb("lt", [128, 2, 64])
    ls = fw.sb("ls", [128, 4])
    fw.op("dve", [lv], [lt], lambda h: h.tensor_tensor(out=lt[:, 0, :], in0=lv[:, 0, :], in1=lv[:, 1, :], op=ALU.mult))
    fw.op("dve", [lv], [lt], lambda h: h.tensor_tensor(out=lt[:, 1, :], in0=lv[:, 2, :], in1=lv[:, 3, :], op=ALU.mult))
    fw.op("dve", [lt], [ls], lambda h: h.reduce_sum(out=ls[:, 0:2], in_=lt[:, :, :], axis=mybir.AxisListType.X))
    fw.op("act", [ls], [ls], lambda h: h.activation(out=ls[:, 0:2], in_=ls[:, 0:2], func=ACT.Exp))
    fw.op("dve", [ls], [ls], lambda h: h.tensor_tensor(out=ls[:, 2:3], in0=ls[:, 1:2], in1=ls[:, 0:1], op=ALU.subtract))
    fw.op("dve", [ls], [ls], lambda h: h.tensor_scalar(out=ls[:, 2:3], in0=ls[:, 2:3], scalar1=-0.2, scalar2=None, op0=ALU.add))
    fw.op("dve", [gs], [gs], lambda h: h.tensor_scalar(out=gs[:, :], in0=gs[:, :], scalar1=0.8, scalar2=None, op0=ALU.mult))
    qTs = Ring([fw.sb("qT%d" % i, [128, S], dma=True) for i in range(2)])
    kTs = Ring([fw.sb("kT%d" % i, [128, 2, S], dma=True) for i in range(2)])
    for kT in kTs.bufs:
        fw.op("pool", [], [kT], lambda h, kT=kT: h.memset(kT[64:128, 0, :], 0.0))
        fw.op("pool", [], [kT], lambda h, kT=kT: h.memset(kT[0:64, 1, :], 0.0))
    vas = Ring([fw.sb("va%d" % i, [128, NT, 130], dma=True) for i in range(2)])
    for va in vas.bufs:
        fw.op("pool", [], [va], lambda h, va=va: h.memset(va[:, :, :], 1.0))
    Ps = Ring([fw.sb("P%d" % i, [128, 256]) for i in range(3)])
    pss = Ring([fw.psum("ps%d" % i, [128, 512]) for i in range(3)])
    accs = Ring([fw.psum("acc%d" % i, [128, 512]) for i in range(2)])
    rr = fw.sb("rr", [128, 4])
    o1 = fw.sb("o1", [128, 128])
    junk = fw.sb("junk", [128, 128])
    ss = fw.sb("ss", [128, 1])
    ots = Ring([fw.sb("ot%d" % i, [128, 128], dma=True) for i in range(2)])
    for hd in range(min(4, LIM_H) if B0_STOP >= 2 else 0):
        qT, kT, va = qTs.next(), kTs.next(), vas.next()
        fw.dma(qT, qT[:, :], W["qkT0"], W["qkT0"].ap[hd, :, :])
        fw.dma(kT, kT[0:64, 0, :], W["qkT0"], W["qkT0"].ap[4 + hd, 0:64, :])
        fw.dma(kT, kT[64:128, 1, :], W["qkT0"], W["qkT0"].ap[4 + hd, 64:128, :])
        for n0 in range(0, NT, 8):
            fw.dma(va, va[:, n0:n0 + 8, 0:128], W["v0"], W["v0"].ap[n0 * 128:(n0 + 8) * 128, hd * 128:(hd + 1) * 128].rearrange("(n p) d -> p n d", p=128))
        for qi in range(LIM_Q):
            acc = accs.next()
            qs = slice(qi * 128, (qi + 1) * 128)
            for kb in range(qi + 1):
                ks = slice(kb * 128, (kb + 1) * 128)
                ps, P = pss.next(), Ps.next()
                for m in range(B0_M):
                    fw.op("pe", [qT, kT], [ps], lambda h, m=m, ps=ps, ks=ks: h.matmul(
                        out=ps[:, m * 128:(m + 1) * 128], lhsT=kT[:, m, ks], rhs=qT[:, qs], start=True, stop=True))
                fw.op("act", [ps], [P], lambda h, ps=ps, P=P: h.activation(out=P[:, :], in_=ps[:, 0:256], func=ACT.Exp, scale=0.125))
                if kb == qi:
                    fw.op("dve", [P, cm], [P], lambda h, P=P: h.tensor_tensor(
                        out=P[:, :].rearrange("p (m q) -> p m q", m=2), in0=P[:, :].rearrange("p (m q) -> p m q", m=2),
                        in1=bc_mid(cm[:, :], 2), op=ALU.mult))
                for m in range(2 if B0_STOP >= 3 else 0):
                    fw.op("pe", [P, va], [acc], lambda h, m=m, P=P, kb=kb, acc=acc: h.matmul(
                        out=acc[:, m * 130:(m + 1) * 130], lhsT=P[:, m * 128:(m + 1) * 128], rhs=va[:, kb, :],
                        start=(kb == 0 and m == 0), stop=(kb == qi), skip_group_check=True))
            if B0_STOP < 4:
                continue
            a3 = acc[:, 0:260].rearrange("p (m d) -> p m d", m=2)
            fw.op("dve", [acc], [rr], lambda h, a3=a3: h.reciprocal(out=rr[:, 0:2], in_=a3[:, :, 128]))
            fw.op("dve", [rr, ls], [rr], lambda h: h.tensor_tensor(out=rr[:, 2:3], in0=rr[:, 1:2], in1=ls[:, 2:3], op=ALU.mult))
            fw.op("dve", [acc, rr], [o1], lambda h, a3=a3: h.tensor_scalar(out=o1[:, :], in0=a3[:, 0, 0:128], scalar1=rr[:, 0:1], scalar2=None, op0=ALU.mult))
            fw.op("dve", [acc, rr, o1], [o1], lambda h, a3=a3: h.scalar_tensor_tensor(
                out=o1[:, :], in0=a3[:, 1, 0:128], scalar=rr[:, 2:3], in1=o1[:, :], op0=ALU.mult, op1=ALU.add))
            ot = ots.next()
            rms(fw, o1, o1[:, :], 128, gs, gs[:, :], ot, ot[:, :], ss, junk)
            fw.dma(W["attn0"], W["attn0"].ap[qs, hd * 128:(hd + 1) * 128], ot, ot[:, :])
    fw.end()


def phase_C0(fw, W, C):
    fw.begin()
    dm = fw.sb("dm", [128, 17, 128], dma=True)
    for r0, r1 in ((0, 8), (8, 17)):
        fw.dma(dm, dm[:, r0:r1, :], C["dmaskT"], C["dmaskT"].ap[r0:r1].rearrange("r k q -> k r q"))
    qTs = Ring([fw.sb("qT%d" % i, [128, 2, S], dma=True) for i in range(1)])
    kTs = Ring([fw.sb("kT%d" % i, [128, 4, S], dma=True) for i in range(1)])
    for kT in kTs.bufs:
        for j in range(4):
            zr = slice(64, 128) if j % 2 == 0 else slice(0, 64)
            fw.op("pool", [], [kT], lambda h, kT=kT, j=j, zr=zr: h.memset(kT[zr, j, :], 0.0))
    vas = Ring([fw.sb("va%d" % i, [128, NT, 4, 66], dma=True) for i in range(1)])
    for va in vas.bufs:
        fw.op("pool", [], [va], lambda h, va=va: h.memset(va[:, :, :, :], 1.0))
    Ps = Ring([fw.sb("P%d" % i, [128, 512]) for i in range(3)])
    pss = Ring([fw.psum("ps%d" % i, [128, 512]) for i in range(3)])
    accs = Ring([fw.psum("acc%d" % i, [128, 512]) for i in range(2)])
    rr = fw.sb("rr", [128, 4])
    ots = Ring([fw.sb("ot%d" % i, [128, 4, 64], dma=True) for i in range(2)])
    flip = 0
    for g in range(min(2, LIM_H)):
        qT, kT, va = qTs.next(), kTs.next(), vas.next()
        for j in range(2):
            fw.dma(qT, qT[:, j, :], W["qkT0"], W["qkT0"].ap[8 + 2 * g + j, :, :])
            fw.dma(kT, kT[0:64, 2 * j, :], W["qkT0"], W["qkT0"].ap[12 + 2 * g + j, 0:64, :])
            fw.dma(kT, kT[64:128, 2 * j + 1, :], W["qkT0"], W["qkT0"].ap[12 + 2 * g + j, 64:128, :])
        for j in range(4):
            c0 = 512 + (4 * g + j) * 64
            for n0 in range(0, NT, 8):
                fw.dma(va, va[:, n0:n0 + 8, j, 0:64], W["v0"], W["v0"].ap[n0 * 128:(n0 + 8) * 128, c0:c0 + 64].rearrange("(n p) d -> p n d", p=128))
        for qi in range(LIM_Q):
            acc = accs.next()
            qs = slice(qi * 128, (qi + 1) * 128)
            kb0 = max(0, qi - 16)
            for kb in range(kb0, qi + 1):
                ks = slice(kb * 128, (kb + 1) * 128)
                ps, P = pss.next(), Ps.next()
                for j in range(4):
                    fw.op("pe", [qT, kT], [ps], lambda h, j=j, ps=ps, ks=ks: h.matmul(
                        out=ps[:, j * 128:(j + 1) * 128], lhsT=kT[:, j, ks], rhs=qT[:, j // 2, qs], start=True, stop=True))
                fw.op("act", [ps], [P], lambda h, ps=ps, P=P: h.activation(out=P[:, :], in_=ps[:, :], func=ACT.Exp, scale=0.125))
                eng = "dve" if flip else "pool"
                flip ^= 1
                fw.op(eng, [P, dm], [P], lambda h, P=P, kb=kb: h.tensor_tensor(
                    out=P[:, :].rearrange("p (m q) -> p m q", m=4), in0=P[:, :].rearrange("p (m q) -> p m q", m=4),
                    in1=bc_mid(dm[:, qi - kb, :], 4), op=ALU.mult))
                for j in range(4):
                    fw.op("pe", [P, va], [acc], lambda h, j=j, P=P, kb=kb, acc=acc: h.matmul(
                        out=acc[:, j * 66:(j + 1) * 66], lhsT=P[:, j * 128:(j + 1) * 128], rhs=va[:, kb, j, :],
                        start=(kb == kb0 and j == 0), stop=(kb == qi), skip_group_check=True))
            a3 = acc[:, 0:264].rearrange("p (m d) -> p m d", m=4)
            fw.op("dve", [acc], [rr], lambda h, a3=a3: h.reciprocal(out=rr[:, 0:4], in_=a3[:, :, 64]))
            ot = ots.next()
            fw.op("dve", [acc, rr], [ot], lambda h, a3=a3, ot=ot: h.tensor_tensor(
                out=ot[:, :, :], in0=a3[:, :, 0:64], in1=bc_last(rr[:, 0:4], 64), op=ALU.mult))
            c0 = 512 + g * 256
            fw.dma(W["attn0"], W["attn0"].ap[qs, c0:c0 + 256], ot, ot[:, :, :].rearrange("p m d -> p (m d)"))
    fw.end()


def phase_D(fw, W, C, attn, wname, li, hin, hout):
    fw.begin()
    w = fw.sb("w", [128, 8, D], dma=True)
    fw.dma(w, w[:, :, :], W[wname], W[wname].ap.rearrange("(k p) n -> p k n", p=128))
    g = fw.sb("g", [128, D], dma=True)
    load_bcast(fw, g, W["g_mix_post"], W["g_mix_post"].ap[li:li + 1, :], D)
    ident = fw.sb("ident", [128, 128], dma=True)
    fw.dma(ident, ident[:, :], C["ident"], C["ident"].ap)
    ats = Ring([fw.sb("at%d" % i, [128, D], dma=True) for i in range(2)])
    hts = Ring([fw.sb("ht%d" % i, [128, D], dma=True) for i in range(2)])
    aT = fw.sb("aT", [128, 8, 128])
    yn = fw.sb("yn", [128, D])
    junk = fw.sb("junk", [128, D])
    ss = fw.sb("ss", [128, 1])
    pT = fw.psum("pT", [128, 1024])
    py = fw.psum("py", [128, 1024])
    for t in range(min(NT, LIM_T)):
        r = slice(t * 128, (t + 1) * 128)
        at, ht = ats.next(), hts.next()
        fw.dma(at, at[:, :], attn, attn.ap[r, :])
        fw.dma(ht, ht[:, :], hin, hin.ap[r, :])
        transposes(fw, at, [at[:, k * 128:(k + 1) * 128] for k in range(8)], pT,
                   [pT[:, k * 128:(k + 1) * 128] for k in range(8)], ident[:, :], ident)
        fw.op("act", [pT], [aT], lambda h: h.copy(out=aT[:, :, :], in_=pT[:, :].rearrange("p (k t) -> p k t", k=8)))
        for c in range(2):
            for k in range(8):
                fw.op("pe", [aT, w], [py], lambda h, k=k, c=c: h.matmul(
                    out=py[:, c * 512:(c + 1) * 512], lhsT=aT[:, k, :], rhs=w[:, k, c * 512:(c + 1) * 512],
                    start=(k == 0), stop=(k == 7)))
        rms(fw, py, py[:, :], D, g, g[:, :], yn, yn[:, :], ss, junk)
        fw.op("pool", [ht, yn], [ht], lambda h, ht=ht: h.tensor_tensor(out=ht[:, :], in0=ht[:, :], in1=yn[:, :], op=ALU.add))
        fw.dma(hout, hout.ap[r, :], ht, ht[:, :])
    fw.end()


def phase_E(fw, W, C, li, hin, hout):
    fw.begin()
    win = fw.sb("win", [128, 8, 4096], BF16)
    wout = fw.sb("wout", [128, 32, D], BF16)
    stg = Ring([fw.sb("stg%d" % i, [128, 2048], dma=True) for i in range(2)])
    n = 0
    for k in range(8):
        for c in range(2):
            s = stg.next()
            fw.dma(s, s[:, :], W["w_mlp_in"], W["w_mlp_in"].ap[li, k * 128:(k + 1) * 128, c * 2048:(c + 1) * 2048])
            fw.op(("dve", "act")[n % 2], [s], [win], (lambda h, s=s, k=k, c=c: h.tensor_copy(out=win[:, k, c * 2048:(c + 1) * 2048], in_=s[:, :])) if n % 2 == 0 else (lambda h, s=s, k=k, c=c: h.copy(out=win[:, k, c * 2048:(c + 1) * 2048], in_=s[:, :])))
            n += 1
    for k2 in range(16):
        s = stg.next()
        fw.dma(s, s[:, :].rearrange("p (a n) -> p a n", a=2), W["w_mlp_out"],
               W["w_mlp_out"].ap[li, k2 * 256:(k2 + 1) * 256, :].rearrange("(a p) n -> p a n", p=128))
        fw.op(("dve", "act")[n % 2], [s], [wout], (lambda h, s=s, k2=k2: h.tensor_copy(
            out=wout[:, 2 * k2:2 * k2 + 2, :], in_=s[:, :].rearrange("p (a n) -> p a n", a=2))) if n % 2 == 0 else (lambda h, s=s, k2=k2: h.copy(
            out=wout[:, 2 * k2:2 * k2 + 2, :], in_=s[:, :].rearrange("p (a n) -> p a n", a=2))))
        n += 1
    gpre = fw.sb("gpre", [128, D], dma=True)
    load_bcast(fw, gpre, W["g_mlp_pre"], W["g_mlp_pre"].ap[li:li + 1, :], D)
    gpost = fw.sb("gpost", [128, D], dma=True)
    load_bcast(fw, gpost, W["g_mlp_post"], W["g_mlp_post"].ap[li:li + 1, :], D)
    ident = fw.sb("ident", [128, 128], dma=True)
    fw.dma(ident, ident[:, :], C["ident"], C["ident"].ap)
    hts = Ring([fw.sb("ht%d" % i, [128, D], dma=True) for i in range(2)])
    hn = fw.sb("hn", [128, D])
    hnT = fw.sb("hnT", [128, 8, 128], BF16)
    hid = fw.sb("hid", [128, 32, 128], BF16)
    rl = Ring([fw.sb("rl%d" % i, [128, 512]) for i in range(2)])
    yn = fw.sb("yn", [128, D])
    junk = fw.sb("junk", [128, D])
    ss = fw.sb("ss", [128, 1])
    pT = fw.psum("pT", [128, 1024])
    pus = Ring([fw.psum("pu%d" % i, [128, 512]) for i in range(3)])
    py = fw.psum("py", [128, 1024])
    for t in range(min(NT, LIM_T)):
        r = slice(t * 128, (t + 1) * 128)
        ht = hts.next()
        fw.dma(ht, ht[:, :], hin, hin.ap[r, :])
        rms(fw, ht, ht[:, :], D, gpre, gpre[:, :], hn, hn[:, :], ss, junk)
        transposes(fw, hn, [hn[:, k * 128:(k + 1) * 128] for k in range(8)], pT,
                   [pT[:, k * 128:(k + 1) * 128] for k in range(8)], ident[:, :], ident)
        fw.op("act", [pT], [hnT], lambda h: h.copy(out=hnT[:, :, :], in_=pT[:, :].rearrange("p (k t) -> p k t", k=8)))
        for f4 in range(8):
            pu = pus.next()
            for f in range(4):
                fc = f4 * 4 + f
                for k in range(8):
                    fw.op("pe", [hnT, win], [pu], lambda h, k=k, f=f, fc=fc, pu=pu: h.matmul(
                        out=pu[:, f * 128:(f + 1) * 128], lhsT=win[:, k, fc * 128:(fc + 1) * 128], rhs=hnT[:, k, :],
                        start=(k == 0), stop=(k == 7)))
            rt = rl.next()
            fw.op("act", [pu], [rt], lambda h, pu=pu, rt=rt: h.activation(out=rt[:, :], in_=pu[:, :], func=ACT.Relu))
            fw.op("dve", [rt], [hid], lambda h, rt=rt, f4=f4: h.tensor_tensor(
                out=hid[:, f4 * 4:(f4 + 1) * 4, :].rearrange("p a t -> p (a t)"), in0=rt[:, :], in1=rt[:, :], op=ALU.mult))
        for c in range(2):
            for k in range(32):
                fw.op("pe", [hid, wout], [py], lambda h, k=k, c=c: h.matmul(
                    out=py[:, c * 512:(c + 1) * 512], lhsT=hid[:, k, :], rhs=wout[:, k, c * 512:(c + 1) * 512],
                    start=(k == 0), stop=(k == 31)))
        rms(fw, py, py[:, :], D, gpost, gpost[:, :], yn, yn[:, :], ss, junk)
        fw.op("pool", [ht, yn], [ht], lambda h, ht=ht: h.tensor_tensor(out=ht[:, :], in0=ht[:, :], in1=yn[:, :], op=ALU.add))
        fw.dma(hout, hout.ap[r, :], ht, ht[:, :])
    fw.end()


def phase_F(fw, W, C, P_, li, hin, hout):
    fw.begin()
    wg = fw.sb("wg", [128, 8, D], dma=True)
    fw.dma(wg, wg[:, :, :], W["w_ple_gate"], W["w_ple_gate"].ap[li].rearrange("(k p) n -> p k n", p=128))
    wp = fw.sb("wp", [128, 2, D], dma=True)
    fw.dma(wp, wp[:, :, :], W["w_ple_proj"], W["w_ple_proj"].ap[li].rearrange("(k p) n -> p k n", p=128))
    ident = fw.sb("ident", [128, 128], dma=True)
    fw.dma(ident, ident[:, :], C["ident"], C["ident"].ap)
    hts = Ring([fw.sb("ht%d" % i, [128, D], dma=True) for i in range(2)])
    pts = Ring([fw.sb("pt%d" % i, [128, 256], dma=True) for i in range(2)])
    hT = fw.sb("hT", [128, 10, 128])
    gate = fw.sb("gate", [128, D])
    pT = fw.psum("pT", [128, 1536])
    pg = fw.psum("pg", [128, 1024])
    pp = fw.psum("pp", [128, 1024])
    for t in range(min(NT, LIM_T)):
        r = slice(t * 128, (t + 1) * 128)
        ht, pt = hts.next(), pts.next()
        fw.dma(ht, ht[:, :], hin, hin.ap[r, :])
        fw.dma(pt, pt[:, :], P_, P_.ap[li, r, :])
        transposes(fw, ht, [ht[:, k * 128:(k + 1) * 128] for k in range(8)], pT,
                   [pT[:, k * 128:(k + 1) * 128] for k in range(8)], ident[:, :], ident)
        transposes(fw, pt, [pt[:, k * 128:(k + 1) * 128] for k in range(2)], pT,
                   [pT[:, (8 + k) * 128:(9 + k) * 128] for k in range(2)], ident[:, :], ident)
        fw.op("act", [pT], [hT], lambda h: h.copy(out=hT[:, :, :], in_=pT[:, 0:1280].rearrange("p (k t) -> p k t", k=10)))
        for c in range(2):
            for k in range(8):
                fw.op("pe", [hT, wg], [pg], lambda h, k=k, c=c: h.matmul(
                    out=pg[:, c * 512:(c + 1) * 512], lhsT=hT[:, k, :], rhs=wg[:, k, c * 512:(c + 1) * 512],
                    start=(k == 0), stop=(k == 7)))
        for c in range(2):
            for k in range(2):
                fw.op("pe", [hT, wp], [pp], lambda h, k=k, c=c: h.matmul(
                    out=pp[:, c * 512:(c + 1) * 512], lhsT=hT[:, 8 + k, :], rhs=wp[:, k, c * 512:(c + 1) * 512],
                    start=(k == 0), stop=(k == 1)))
        fw.op("act", [pg], [gate], lambda h: h.activation(out=gate[:, :], in_=pg[:, :], func=ACT.Sigmoid))
        fw.op("dve", [pp, gate], [gate], lambda h: h.tensor_tensor(out=gate[:, :], in0=pp[:, :], in1=gate[:, :], op=ALU.mult))
        fw.op("pool", [ht, gate], [ht], lambda h, ht=ht: h.tensor_tensor(out=ht[:, :], in0=ht[:, :], in1=gate[:, :], op=ALU.add))
        fw.dma(hout, hout.ap[r, :], ht, ht[:, :])
    fw.end()


def phase_A1(fw, W, C, hin):
    fw.begin()
    wc = fw.sb("wc", [128, 8, 744], dma=True)
    fw.dma(wc, wc[:, :, :], W["w_in_c"], W["w_in_c"].ap.rearrange("(k p) n -> p k n", p=128))
    wuq = fw.sb("wuq", [128, 3, 1536], dma=True)
    fw.dma(wuq, wuq[:, :, :], W["w_uq"], W["w_uq"].ap.rearrange("(k p) n -> p k n", p=128))
    wqi = fw.sb("wqi", [128, 3, 512], dma=True)
    fw.dma(wqi, wqi[:, :, :], W["w_qi"], W["w_qi"].ap.rearrange("(k p) n -> p k n", p=128))
    wuk = fw.sb("wuk", [128, 2, 1024], dma=True)
    fw.dma(wuk, wuk[:, :, :], W["w_uk"], W["w_uk"].ap.rearrange("(k p) n -> p k n", p=128))
    wuv = fw.sb("wuv", [128, 2, 1024], dma=True)
    fw.dma(wuv, wuv[:, :, :], W["w_uv"], W["w_uv"].ap.rearrange("(k p) n -> p k n", p=128))
    g = fw.sb("g", [128, D], dma=True)
    load_bcast(fw, g, W["g_mix_pre"], W["g_mix_pre"].ap[1:2, :], D)
    gq = fw.sb("gq", [128, 384], dma=True)
    load_bcast(fw, gq, W["g_cq"], W["g_cq"].ap[0:1, :], 384)
    gkv = fw.sb("gkv", [128, 256], dma=True)
    load_bcast(fw, gkv, W["g_ckv"], W["g_ckv"].ap[0:1, :], 256)
    ident = fw.sb("ident", [128, 128], dma=True)
    fw.dma(ident, ident[:, :], C["ident"], C["ident"].ap)
    pA = fw.psum("pA", [128, 2048])
    pB = fw.psum("pB", [128, 2048])
    wukT = fw.sb("wukT", [64, 16, 256])
    for cc in range(2):
        transposes(fw, wuk, [wuk[:, cc, hh * 64:(hh + 1) * 64] for hh in range(16)], pA,
                   [pA[0:64, hh * 128:(hh + 1) * 128] for hh in range(16)], ident[:, :], ident)
        fw.op("act", [pA], [wukT], lambda h, cc=cc: h.copy(
            out=wukT[:, :, cc * 128:(cc + 1) * 128], in_=pA[0:64, :].rearrange("p (h c) -> p h c", h=16)))
    hts = Ring([fw.sb("ht%d" % i, [128, D], dma=True) for i in range(2)])
    tabs = Ring([fw.sb("tab%d" % i, [128, 32], dma=True) for i in range(2)])
    hn = fw.sb("hn", [128, D])
    hnT = fw.sb("hnT", [128, 8, 128])
    junk = fw.sb("junk", [128, D])
    ss = fw.sb("ss", [128, 1])
    csb = fw.sb("csb", [128, 744])
    cqn = fw.sb("cqn", [128, 384])
    cqT = fw.sb("cqT", [128, 3, 128])
    kvtok = fw.sb("kvtok", [128, 288])
    kis = fw.sb("kis", [128, 64])
    tmp = fw.sb("tmp", [128, 2, 16, 16])
    wis = Ring([fw.sb("wis%d" % i, [128, 8], dma=True) for i in range(2)])
    qn = fw.sb("qn", [128, 16, 64])
    qsb = fw.sb("qsb", [128, 1536])
    qr = fw.sb("qr", [128, 16, 32])
    qnT = fw.sb("qnT", [64, 16, 128])
    qfTs = Ring([fw.sb("qfT%d" % i, [128, 3, 2048], dma=True) for i in range(1)])
    kvTs = Ring([fw.sb("kvT%d" % i, [128, 3, 128], dma=True) for i in range(2)])
    vhs = Ring([fw.sb("vh%d" % i, [128, D], dma=True) for i in range(2)])
    qis = fw.sb("qis", [128, 8, 64])
    qiTs = Ring([fw.sb("qiT%d" % i, [64, 8, 128], dma=True) for i in range(2)])
    kiTs = Ring([fw.sb("kiT%d" % i, [64, 128], dma=True) for i in range(2)])
    for t in range(min(NT, LIM_T) if A1_STOP >= 2 else 0):
        r = slice(t * 128, (t + 1) * 128)
        ht, tab = hts.next(), tabs.next()
        fw.dma(ht, ht[:, :], hin, hin.ap[r, :])
        fw.dma(tab, tab[:, :], C["tab32"], C["tab32"].ap[r, :])
        cos, sin = tab[:, 0:16], tab[:, 16:32]
        rms(fw, ht, ht[:, :], D, g, g[:, :], hn, hn[:, :], ss, junk)
        transposes(fw, hn, [hn[:, k * 128:(k + 1) * 128] for k in range(8)], pA,
                   [pA[:, k * 128:(k + 1) * 128] for k in range(8)], ident[:, :], ident)
        fw.op("act", [pA], [hnT], lambda h: h.copy(out=hnT[:, :, :], in_=pA[:, 0:1024].rearrange("p (k t) -> p k t", k=8)))
        for c, (c0, cw) in enumerate(((0, 512), (512, 232))):
            for k in range(8):
                fw.op("pe", [hnT, wc], [pB], lambda h, k=k, c=c, c0=c0, cw=cw: h.matmul(
                    out=pB[:, c * 512:c * 512 + cw], lhsT=hnT[:, k, :], rhs=wc[:, k, c0:c0 + cw], start=(k == 0), stop=(k == 7)))
        fw.op("act", [pB], [csb], lambda h: h.copy(out=csb[:, 0:512], in_=pB[:, 0:512]))
        fw.op("dve", [pB], [csb], lambda h: h.tensor_copy(out=csb[:, 512:744], in_=pB[:, 512:744]))
        rms(fw, csb, csb[:, 0:384], 384, gq, gq[:, :], cqn, cqn[:, :], ss, junk)
        rms(fw, csb, csb[:, 384:640], 256, gkv, gkv[:, :], kvtok, kvtok[:, 0:256], ss, junk)
        rope_emit(fw, csb, csb[:, 640:672].rearrange("p (h d) -> p h d", h=1), kvtok,
                  kvtok[:, 256:288].rearrange("p (h d) -> p h d", h=1), tab, cos, sin, 1, 16,
                  tmp, tmp[:, 0, 0:1, :], tmp[:, 1, 0:1, :])
        rope_emit(fw, csb, csb[:, 672:704].rearrange("p (h d) -> p h d", h=1), kis,
                  kis[:, 0:32].rearrange("p (h d) -> p h d", h=1), tab, cos, sin, 1, 16,
                  tmp, tmp[:, 0, 0:1, :], tmp[:, 1, 0:1, :])
        fw.op("act", [csb], [kis], lambda h: h.copy(out=kis[:, 32:64], in_=csb[:, 704:736]))
        wi = wis.next()
        fw.op("act", [csb], [wi], lambda h, wi=wi: h.mul(out=wi[:, :], in_=csb[:, 736:744], mul=float(8.0 ** -0.5 * 64.0 ** -0.5)))
        fw.dma(W["wi"], W["wi"].ap[r, :], wi, wi[:, :])
        if A1_STOP < 3:
            continue
        transposes(fw, cqn, [cqn[:, k * 128:(k + 1) * 128] for k in range(3)], pA,
                   [pA[:, 1024 + k * 128:1024 + (k + 1) * 128] for k in range(3)], ident[:, :], ident)
        fw.op("act", [pA], [cqT], lambda h: h.copy(out=cqT[:, :, :], in_=pA[:, 1024:1408].rearrange("p (k t) -> p k t", k=3)))
        for c in range(3):
            for k in range(3):
                fw.op("pe", [cqT, wuq], [pB], lambda h, k=k, c=c: h.matmul(
                    out=pB[:, c * 512:(c + 1) * 512], lhsT=cqT[:, k, :], rhs=wuq[:, k, c * 512:(c + 1) * 512],
                    start=(k == 0), stop=(k == 2)))
        fw.op("act", [pB], [qsb], lambda h: h.copy(out=qsb[:, 0:1024], in_=pB[:, 0:1024]))
        fw.op("dve", [pB], [qsb], lambda h: h.tensor_copy(out=qsb[:, 1024:1536], in_=pB[:, 1024:1536]))
        q3 = qsb[:, :].rearrange("p (h d) -> p h d", h=16)
        fw.op("act", [qsb], [qn], lambda h, q3=q3: h.copy(out=qn[:, :, :], in_=q3[:, :, 0:64]))
        rope_emit(fw, qsb, q3[:, :, 64:96], qr, qr[:, :, :], tab, cos, sin, 16, 16, tmp, tmp[:, 0, :, :], tmp[:, 1, :, :])
        for k in range(3):
            fw.op("pe", [cqT, wqi], [pB], lambda h, k=k: h.matmul(
                out=pB[:, 1536:2048], lhsT=cqT[:, k, :], rhs=wqi[:, k, :], start=(k == 0), stop=(k == 2)))
        qi3 = pB[:, 1536:2048].rearrange("p (h d) -> p h d", h=8)
        rope_emit(fw, pB, qi3[:, :, 0:32], qis, qis[:, :, 0:32], tab, cos, sin, 8, 16, tmp, tmp[:, 0, 0:8, :], tmp[:, 1, 0:8, :])
        fw.op("act", [pB], [qis], lambda h, qi3=qi3: h.copy(out=qis[:, :, 32:64], in_=qi3[:, :, 32:64]))
        if A1_STOP < 4:
            continue
        transposes(fw, qn, [qn[:, hh, :] for hh in range(16)], pA,
                   [pA[0:64, hh * 128:(hh + 1) * 128] for hh in range(16)], ident[:, :], ident)
        fw.op("act", [pA], [qnT], lambda h: h.copy(out=qnT[:, :, :], in_=pA[0:64, :].rearrange("p (h t) -> p h t", h=16)))
        qfT = qfTs.next()
        for cc in range(2):
            for hh in range(16):
                fw.op("pe", [qnT, wukT], [pA], lambda h, hh=hh, cc=cc: h.matmul(
                    out=pA[:, hh * 128:(hh + 1) * 128], lhsT=wukT[:, hh, cc * 128:(cc + 1) * 128], rhs=qnT[:, hh, :],
                    start=True, stop=True))
            fw.op(("act", "dve")[cc], [pA], [qfT], (lambda h, qfT=qfT: h.copy(out=qfT[:, 0, :], in_=pA[:, :])) if cc == 0 else
                  (lambda h, qfT=qfT: h.tensor_copy(out=qfT[:, 1, :], in_=pA[:, :])))
        transposes(fw, qr, [qr[:, hh, :] for hh in range(16)], pA,
                   [pA[0:32, hh * 128:(hh + 1) * 128] for hh in range(16)], ident[:, :], ident)
        fw.op("act", [pA], [qfT], lambda h, qfT=qfT: h.copy(out=qfT[0:32, 2, :], in_=pA[0:32, :]))
        fw.dma(W["qfT"], W["qfT"].ap[t, 0:256, :].rearrange("(c p) n -> p c n", p=128), qfT, qfT[:, 0:2, :])
        fw.dma(W["qfT"], W["qfT"].ap[t, 256:288, :], qfT, qfT[0:32, 2, :])
        if A1_STOP < 5:
            continue
        kvT = kvTs.next()
        transposes(fw, kvtok, [kvtok[:, 0:128], kvtok[:, 128:256]], pA,
                   [pA[:, 0:128], pA[:, 128:256]], ident[:, :], ident)
        transposes(fw, kvtok, [kvtok[:, 256:288]], pA, [pA[0:32, 256:384]], ident[:, :], ident)
        fw.op("act", [pA], [kvT], lambda h, kvT=kvT: h.copy(out=kvT[:, 0:2, :], in_=pA[:, 0:256].rearrange("p (k t) -> p k t", k=2)))
        fw.op("dve", [pA], [kvT], lambda h, kvT=kvT: h.tensor_copy(out=kvT[0:32, 2, :], in_=pA[0:32, 256:384]))
        fw.dma(W["kvT"], W["kvT"].ap[0:256, r].rearrange("(c p) s -> p c s", p=128), kvT, kvT[:, 0:2, :])
        fw.dma(W["kvT"], W["kvT"].ap[256:288, r], kvT, kvT[0:32, 2, :])
        for c in range(2):
            for k in range(2):
                fw.op("pe", [kvT, wuv], [pB], lambda h, k=k, c=c, kvT=kvT: h.matmul(
                    out=pB[:, c * 512:(c + 1) * 512], lhsT=kvT[:, k, :], rhs=wuv[:, k, c * 512:(c + 1) * 512],
                    start=(k == 0), stop=(k == 1)))
        vh = vhs.next()
        fw.op("act", [pB], [vh], lambda h, vh=vh: h.copy(out=vh[:, :], in_=pB[:, 0:1024]))
        fw.dma(W["v1"], W["v1"].ap[r, :], vh, vh[:, :])
        if A1_STOP < 6:
            continue
        transposes(fw, qis, [qis[:, hh, :] for hh in range(8)], pA,
                   [pA[0:64, 512 + hh * 128:512 + (hh + 1) * 128] for hh in range(8)], ident[:, :], ident)
        transposes(fw, kis, [kis[:, :]], pA, [pA[0:64, 1536:1664]], ident[:, :], ident)
        qiT, kiT = qiTs.next(), kiTs.next()
        fw.op("act", [pA], [qiT], lambda h, qiT=qiT: h.copy(out=qiT[:, :, :], in_=pA[0:64, 512:1536].rearrange("p (h t) -> p h t", h=8)))
        fw.op("dve", [pA], [kiT], lambda h, kiT=kiT: h.tensor_copy(out=kiT[:, :], in_=pA[0:64, 1536:1664]))
        fw.dma(W["qiT"], W["qiT"].ap[t, :, :], qiT, qiT[:, :, :].rearrange("p h t -> p (h t)"))
        fw.dma(W["kiT"], W["kiT"].ap[:, r], kiT, kiT[:, :])
    fw.end()


def phase_B1a(fw, W, C):
    fw.begin()
    ident = fw.sb("ident", [128, 128], dma=True)
    fw.dma(ident, ident[:, :], C["ident"], C["ident"].ap)
    cb = fw.sb("cb", [128, 128], dma=True)
    fw.dma(cb, cb[:, :], C["cbias"], C["cbias"].ap)
    kiT = fw.sb("kiT", [64, S], dma=True)
    fw.dma(kiT, kiT[:, :], W["kiT"], W["kiT"].ap)
    qiTs = Ring([fw.sb("qiT%d" % i, [64, 8, 128], dma=True) for i in range(2)])
    wis = Ring([fw.sb("wi%d" % i, [128, 8], dma=True) for i in range(2)])
    score = fw.sb("score", [128, S])
    work = fw.sb("work", [128, S])
    m8 = fw.sb("m8", [128, 8])
    rls = Ring([fw.sb("rl%d" % i, [128, 512]) for i in range(3)])
    mTs = Ring([fw.sb("mT%d" % i, [128, NT, 128], dma=True) for i in range(2)])
    pss = Ring([fw.psum("ps%d" % i, [128, 512]) for i in range(4)])
    pts = Ring([fw.psum("pt%d" % i, [128, 512]) for i in range(2)])
    for qi in range(LIM_Q):
        n = (qi + 1) * 128
        qiT, wi = qiTs.next(), wis.next()
        fw.dma(qiT, qiT[:, :, :].rearrange("p h t -> p (h t)"), W["qiT"], W["qiT"].ap[qi, :, :])
        fw.dma(wi, wi[:, :], W["wi"], W["wi"].ap[qi * 128:(qi + 1) * 128, :])
        for c0 in range(0, n, 512):
            cw = min(512, n - c0)
            for hh in range(8):
                ps, rl = pss.next(), rls.next()
                fw.op("pe", [qiT, kiT], [ps], lambda h, hh=hh, ps=ps, c0=c0, cw=cw, qiT=qiT: h.matmul(
                    out=ps[:, 0:cw], lhsT=qiT[:, hh, :], rhs=kiT[:, c0:c0 + cw], start=True, stop=True))
                fw.op("act", [ps], [rl], lambda h, ps=ps, rl=rl, cw=cw: h.activation(out=rl[:, 0:cw], in_=ps[:, 0:cw], func=ACT.Relu))
                if hh == 0:
                    fw.op("dve", [rl, wi], [score], lambda h, rl=rl, wi=wi, c0=c0, cw=cw: h.tensor_scalar(
                        out=score[:, c0:c0 + cw], in0=rl[:, 0:cw], scalar1=wi[:, 0:1], scalar2=None, op0=ALU.mult))
                else:
                    fw.op("dve", [rl, wi, score], [score], lambda h, rl=rl, wi=wi, c0=c0, cw=cw, hh=hh: h.scalar_tensor_tensor(
                        out=score[:, c0:c0 + cw], in0=rl[:, 0:cw], scalar=wi[:, hh:hh + 1], in1=score[:, c0:c0 + cw],
                        op0=ALU.mult, op1=ALU.add))
        fw.op("dve", [score, cb], [score], lambda h, n=n: h.tensor_tensor(
            out=score[:, n - 128:n], in0=score[:, n - 128:n], in1=cb[:, :], op=ALU.add))
        if qi < 2:
            fw.op("dve", [score], [work], lambda h, n=n: h.tensor_scalar(
                out=work[:, 0:n], in0=score[:, 0:n], scalar1=-1.0e29, scalar2=None, op0=ALU.is_ge))
        else:
            src = score
            for it in range(32):
                fw.op("dve", [src], [m8], lambda h, src=src, n=n: h.max(out=m8[:, :], in_=src[:, 0:n]))
                if it < 31:
                    fw.op("dve", [src, m8], [work], lambda h, src=src, n=n: h.match_replace(
                        out=work[:, 0:n], in_to_replace=m8[:, :], in_values=src[:, 0:n], imm_value=NEG))
                    src = work
            fw.op("dve", [score, m8], [work], lambda h, n=n: h.tensor_scalar(
                out=work[:, 0:n], in0=score[:, 0:n], scalar1=m8[:, 7:8], scalar2=None, op0=ALU.is_ge))
        mT = mTs.next()
        for k0 in range(0, qi + 1, 4):
            kn = min(4, qi + 1 - k0)
            pt = pts.next()
            transposes(fw, work, [work[:, (k0 + j) * 128:(k0 + j + 1) * 128] for j in range(kn)], pt,
                       [pt[:, j * 128:(j + 1) * 128] for j in range(kn)], ident[:, :], ident)
            fw.op("act", [pt], [mT], lambda h, pt=pt, mT=mT, k0=k0, kn=kn: h.copy(
                out=mT[:, k0:k0 + kn, :], in_=pt[:, 0:kn * 128].rearrange("p (k q) -> p k q", k=kn)))
        for k0 in range(0, qi + 1, 8):
            k1 = min(qi + 1, k0 + 8)
            fw.dma(W["maskT"], W["maskT"].ap[qi, :, k0:k1, :], mT, mT[:, k0:k1, :])
    fw.end()


def phase_B1b(fw, W, C):
    fw.begin()
    kvT = fw.sb("kvT", [128, 3, S], BF16)
    stg = Ring([fw.sb("stg%d" % i, [128, 3, 512], dma=True) for i in range(2)])
    for st in stg.bufs:
        fw.op("pool", [], [st], lambda h, st=st: h.memset(st[:, :, :], 0.0))
    for c0 in range(0, S, 512):
        st = stg.next()
        fw.dma(st, st[:, 0:2, :], W["kvT"], W["kvT"].ap[0:256, c0:c0 + 512].rearrange("(c p) s -> p c s", p=128))
        fw.dma(st, st[0:32, 2, :], W["kvT"], W["kvT"].ap[256:288, c0:c0 + 512])
        fw.op(("dve", "act")[(c0 // 512) % 2], [st], [kvT], (lambda h, st=st, c0=c0: h.tensor_copy(out=kvT[:, :, c0:c0 + 512], in_=st[:, :, :]))
              if (c0 // 512) % 2 == 0 else (lambda h, st=st, c0=c0: h.copy(out=kvT[:, :, c0:c0 + 512], in_=st[:, :, :])))
    qfs = Ring([fw.sb("qf%d" % i, [128, 3, 2048], dma=True) for i in range(1)])
    for qf in qfs.bufs:
        fw.op("pool", [], [qf], lambda h, qf=qf: h.memset(qf[:, 2, :], 0.0))
    qfTs = Ring([fw.sb("qfT%d" % i, [128, 3, 2048], BF16) for i in range(2)])
    mTs = Ring([fw.sb("mT%d" % i, [128, NT, 128], dma=True) for i in range(2)])
    vas = Ring([fw.sb("va%d" % i, [128, 16, 66], dma=True) for i in range(3)])
    for va in vas.bufs:
        fw.op("pool", [], [va], lambda h, va=va: h.memset(va[:, :, :], 1.0))
    vbs = Ring([fw.sb("vb%d" % i, [128, 16, 66], BF16) for i in range(3)])
    Ps = Ring([fw.sb("P%d" % i, [128, 512]) for i in range(4)])
    Pbs = Ring([fw.sb("Pb%d" % i, [128, 512], BF16) for i in range(4)])
    pss = Ring([fw.psum("ps%d" % i, [128, 512]) for i in range(4)])
    acc = fw.psum("acc", [128, 2048])
    rr = fw.sb("rr", [128, 16])
    ots = Ring([fw.sb("ot%d" % i, [128, 16, 64], dma=True) for i in range(2)])
    flip = 0
    for qi in range(LIM_Q):
        qs = slice(qi * 128, (qi + 1) * 128)
        qf, qfT, mT = qfs.next(), qfTs.next(), mTs.next()
        fw.dma(qf, qf[:, 0:2, :], W["qfT"], W["qfT"].ap[qi, 0:256, :].rearrange("(c p) n -> p c n", p=128))
        fw.dma(qf, qf[0:32, 2, :], W["qfT"], W["qfT"].ap[qi, 256:288, :])
        fw.op("act", [qf], [qfT], lambda h, qf=qf, qfT=qfT: h.copy(out=qfT[:, 0:2, :], in_=qf[:, 0:2, :]))
        fw.op("dve", [qf], [qfT], lambda h, qf=qf, qfT=qfT: h.tensor_copy(out=qfT[:, 2, :], in_=qf[:, 2, :]))
        for k0 in range(0, qi + 1, 8):
            k1 = min(qi + 1, k0 + 8)
            fw.dma(mT, mT[:, k0:k1, :], W["maskT"], W["maskT"].ap[qi, :, k0:k1, :])
        for kb in range(qi + 1):
            ks = slice(kb * 128, (kb + 1) * 128)
            va, vb = vas.next(), vbs.next()
            fw.dma(va, va[:, :, 0:64], W["v1"], W["v1"].ap[ks, :].rearrange("p (h d) -> p h d", h=16))
            fw.op("act", [va], [vb], lambda h, va=va, vb=vb: h.copy(out=vb[:, :, :], in_=va[:, :, :]))
            for g in range(4):
                ps, P, Pb = pss.next(), Ps.next(), Pbs.next()
                for c in range(3):
                    rows = slice(0, 128) if c < 2 else slice(0, 32)
                    fw.op("pe", [qfT, kvT], [ps], lambda h, c=c, rows=rows, ps=ps, ks=ks, g=g, qfT=qfT: h.matmul(
                        out=ps[:, :], lhsT=kvT[rows, c, ks], rhs=qfT[rows, c, g * 512:(g + 1) * 512],
                        start=(c == 0), stop=(c == 2)))
                fw.op("act", [ps], [P], lambda h, ps=ps, P=P: h.activation(out=P[:, :], in_=ps[:, :], func=ACT.Exp, scale=float(C_SCALE)))
                fw.op("dve", [P, mT], [Pb], lambda h, P=P, Pb=Pb, kb=kb, mT=mT: h.tensor_tensor(
                    out=Pb[:, :].rearrange("p (m q) -> p m q", m=4), in0=P[:, :].rearrange("p (m q) -> p m q", m=4),
                    in1=bc_mid(mT[:, kb, :], 4), op=ALU.mult))
                for j in range(4):
                    hh = g * 4 + j
                    bank, slot = hh // 6, hh % 6
                    o0 = bank * 512 + slot * 66
                    fw.op("pe", [Pb, vb], [acc], lambda h, j=j, Pb=Pb, vb=vb, hh=hh, o0=o0, kb=kb, slot=slot: h.matmul(
                        out=acc[:, o0:o0 + 66], lhsT=Pb[:, j * 128:(j + 1) * 128], rhs=vb[:, hh, :],
                        start=(kb == 0 and slot == 0), stop=(kb == qi), skip_group_check=True))
        ot = ots.next()
        for bank in range(3):
            nh = 6 if bank < 2 else 4
            a3 = acc[:, bank * 512:bank * 512 + nh * 66].rearrange("p (m d) -> p m d", m=nh)
            fw.op("dve", [acc], [rr], lambda h, a3=a3, bank=bank, nh=nh: h.reciprocal(out=rr[:, bank * 6:bank * 6 + nh], in_=a3[:, :, 64]))
            fw.op("dve", [acc, rr], [ot], lambda h, a3=a3, ot=ot, bank=bank, nh=nh: h.tensor_tensor(
                out=ot[:, bank * 6:bank * 6 + nh, :], in0=a3[:, :, 0:64], in1=bc_last(rr[:, bank * 6:bank * 6 + nh], 64), op=ALU.mult))
        fw.dma(W["attn1"], W["attn1"].ap[qs, :], ot, ot[:, :, :].rearrange("p m d -> p (m d)"))
    fw.end()


WEIGHT_SHAPES = {
    "g_mix_pre": [2, D], "g_mix_post": [2, D], "g_mlp_pre": [2, D], "g_mlp_post": [2, D],
    "w_mlp_in": [2, D, 4096], "w_mlp_out": [2, 4096, D], "w_ple_proj": [2, 256, D], "w_ple_gate": [2, D, D],
    "w_in_ab": [D, 3072], "w_out_ab": [D, D], "diff_lq1": [1, 64], "diff_lk1": [1, 64], "diff_lq2": [1, 64],
    "diff_lk2": [1, 64], "g_diff_sub": [1, 128], "w_in_c": [D, 744], "g_cq": [1, 384], "g_ckv": [1, 256],
    "w_uq": [384, 1536], "w_qi": [384, 512], "w_uk": [256, 1024], "w_uv": [256, 1024], "w_out_c": [D, D],
}
CONST_SHAPES = {"ident": [128, 128], "tab64": [S, 64], "tab32": [S, 32], "cmaskT": [128, 128],
                "cbias": [128, 128], "dmaskT": [17, 128, 128]}
SCRATCH_SHAPES = {
    "qkT0": [16, 128, S], "v0": [S, D], "attn0": [S, D], "h0a": [S, D], "h0b": [S, D], "h0c": [S, D],
    "qfT": [NT, 288, 2048], "kvT": [288, S], "v1": [S, D], "qiT": [NT, 64, 1024], "kiT": [64, S], "wi": [S, 8],
    "maskT": [NT, 128, NT, 128], "attn1": [S, D], "h1a": [S, D], "h1b": [S, D],
}


def build(only=None, ext_in=(), ext_out=()):
    nc = bass.Bass("TRN2", target_bir_lowering=False)
    W, C = {}, {}
    X = Buf("x", nc.dram_tensor("x", [S, D], F32, kind="ExternalInput").ap(), accumulate=True)
    P_ = Buf("p", nc.dram_tensor("p", [2, S, 256], F32, kind="ExternalInput").ap(), accumulate=True)
    for k, shp in WEIGHT_SHAPES.items():
        W[k] = Buf(k, nc.dram_tensor(k, shp, F32, kind="ExternalInput").ap(), accumulate=True)
    for k, shp in CONST_SHAPES.items():
        C[k] = Buf(k, nc.dram_tensor(k, shp, F32, kind="ExternalInput").ap(), accumulate=True)
    for k, shp in SCRATCH_SHAPES.items():
        kind = "ExternalInput" if k in ext_in else ("ExternalOutput" if k in ext_out else "Internal")
        W[k] = Buf(k, nc.dram_tensor(k, shp, F32, kind=kind).ap(), accumulate=True)
    OUT = Buf("out", nc.dram_tensor("out", [S, D], F32, kind="ExternalOutput").ap(), accumulate=True)
    phases = [
        lambda: phase_A0(fw, X, W, None, C),
        lambda: phase_B0(fw, W, C),
        lambda: phase_C0(fw, W, C),
        lambda: phase_D(fw, W, C, W["attn0"], "w_out_ab", 0, X, W["h0a"]),
        lambda: phase_E(fw, W, C, 0, W["h0a"], W["h0b"]),
        lambda: phase_F(fw, W, C, P_, 0, W["h0b"], W["h0c"]),
        lambda: phase_A1(fw, W, C, W["h0c"]),
        lambda: phase_B1a(fw, W, C),
        lambda: phase_B1b(fw, W, C),
        lambda: phase_D(fw, W, C, W["attn1"], "w_out_c", 1, W["h0c"], W["h1a"]),
        lambda: phase_E(fw, W, C, 1, W["h1a"], W["h1b"]),
        lambda: phase_F(fw, W, C, P_, 1, W["h1b"], OUT),
    ]
    with ExitStack() as es:
        fw = FW(nc, es)
        for i, ph in enumerate(phases):
            if only is not None and i not in only:
                continue
            ph()
        fw.barrier()
    return nc


def make_consts():
    c = {}
    c["ident"] = np.eye(128, dtype=np.float32)
    pos = np.arange(S, dtype=np.float32)
    for half, nm in ((32, "tab64"), (16, "tab32")):
        inv = (np.float32(10000.0) ** (-np.arange(half, dtype=np.float32) / np.float32(half))).astype(np.float32)
        ang = (pos[:, None] * inv[None, :]).astype(np.float32)
        c[nm] = np.concatenate([np.cos(ang), np.sin(ang)], axis=1).astype(np.float32)
    k = np.arange(128)[:, None]
    q = np.arange(128)[None, :]
    c["cmaskT"] = (k <= q).astype(np.float32)
    c["cbias"] = np.where(np.arange(128)[None, :] <= np.arange(128)[:, None], 0.0, NEG).astype(np.float32)
    dm = np.zeros((17, 128, 128), np.float32)
    for rel in range(17):
        dist = rel * 128 + q - k
        for w_, d_ in ((128, 1), (512, 4), (2048, 16)):
            dm[rel] += ((dist >= 0) & (dist <= w_) & (dist % d_ == 0)).astype(np.float32)
    c["dmaskT"] = dm
    return c


def make_in_maps(inputs, ncores=8):
    consts = make_consts()
    shared = {}
    for k in WEIGHT_SHAPES:
        a = np.ascontiguousarray(np.asarray(inputs[k], dtype=np.float32))
        shared[k] = a.reshape(WEIGHT_SHAPES[k])
    shared.update(consts)
    x = np.asarray(inputs["x"], dtype=np.float32)
    p = np.asarray(inputs["p"], dtype=np.float32)
    maps = []
    for b in range(ncores):
        m = dict(shared)
        m["x"] = np.ascontiguousarray(x[b])
        m["p"] = np.ascontiguousarray(p[:, b])
        maps.append(m)
    return maps


def kernel(**inputs):
    nc = build()
    maps = make_in_maps(inputs, 8)
    res = run_bass_kernel_spmd(nc, maps, core_ids=list(range(8)))
    return np.stack([np.asarray(r["out"], dtype=np.float32) for r in res.results], axis=0)
```

```python
import numpy as np
from contextlib import ExitStack
import concourse.bass as bass
import concourse.mybir as mybir
from concourse.bass_utils import run_bass_kernel_spmd

F32 = mybir.dt.float32
BF16 = mybir.dt.bfloat16
ALU = mybir.AluOpType
ACT = mybir.ActivationFunctionType

S = 4096
NT = S // 128
D = 1024
EPS = 1e-6
NEG = -1.0e30
C_SCALE = 96.0 ** -0.5
LIM_H = 99
LIM_Q = NT
B0_STOP = 4
LIM_T = NT
A1_STOP = 9
B0_M = 2


class Buf:
    def __init__(self, name, ap=None, accumulate=False, dsem=None):
        self.name, self.ap, self.accumulate, self.dsem = name, ap, accumulate, dsem
        self.writers = {}
        self.readers = {}

    def __getitem__(self, k):
        return self.ap[k]


class E:
    def __init__(self, name, h, sem):
        self.name, self.h, self.sem = name, h, sem
        self.count = 0
        self.seen = {}


class FW:
    def __init__(self, nc, es, ndsem=56):
        self.nc = nc
        self.engs = {}
        for name, h in (("pe", nc.tensor), ("dve", nc.vector), ("act", nc.scalar),
                        ("pool", nc.gpsimd), ("sp", nc.sync)):
            self.engs[name] = E(name, h, es.enter_context(nc.semaphore("s_" + name)))
        self.pool_sems = [es.enter_context(nc.semaphore("d%d" % i)) for i in range(ndsem)]
        self.semcnt = {}
        self.ps = None
        self.uid = 0

    def begin(self):
        self.ps = ExitStack()
        self.free_sems = list(self.pool_sems)

    def end(self):
        self.barrier()
        self.ps.close()
        self.ps = None

    def sb(self, name, shape, dt=F32, dma=False):
        self.uid += 1
        t = self.ps.enter_context(self.nc.sbuf_tensor("%s_%d" % (name, self.uid), list(shape), dt))
        b = Buf(name, t.ap())
        if dma:
            b.dsem = self.free_sems.pop()
        return b

    def psum(self, name, shape, dt=F32):
        self.uid += 1
        t = self.ps.enter_context(self.nc.psum_tensor("%s_%d" % (name, self.uid), list(shape), dt))
        return Buf(name, t.ap())

    def _wait(self, e, deps):
        for sem, val in deps.items():
            if e.seen.get(sem, 0) >= val:
                continue
            e.h.wait_ge(sem, val)
            e.seen[sem] = val

    def _deps(self, e, reads, writes, is_dma=False):
        deps = {}

        def add(d):
            for s, v in d.items():
                if deps.get(s, 0) < v:
                    deps[s] = v
        for b in reads:
            add(b.writers)
        for b in writes:
            if b.accumulate:
                continue
            add(b.readers)
            if not (is_dma and b.dsem is not None and list(b.writers.keys()) == [b.dsem]):
                add(b.writers)
        if e.name == "pe":
            deps.pop(e.sem, None)
        return deps

    def _commit(self, reads, writes, sem, val):
        for b in reads:
            if b.readers.get(sem, 0) < val:
                b.readers[sem] = val
        for b in writes:
            if b.accumulate:
                if b.writers.get(sem, 0) < val:
                    b.writers[sem] = val
            else:
                if list(b.writers.keys()) == [sem] and not b.readers:
                    b.writers[sem] = val
                else:
                    b.writers = {sem: val}
                b.readers = {}

    def op(self, eng, reads, writes, fn):
        e = self.engs[eng]
        self._wait(e, self._deps(e, reads, writes))
        ins = fn(e.h)
        e.count += 1
        ins.then_inc(e.sem, 1)
        self._commit(reads, writes, e.sem, e.count)
        return ins

    def dma(self, out_buf, out_ap, in_buf, in_ap, q="sp"):
        e = self.engs[q]
        sbb = out_buf if out_buf.dsem is not None else in_buf
        assert sbb.dsem is not None, (out_buf.name, in_buf.name)
        self._wait(e, self._deps(e, [in_buf], [out_buf], is_dma=True))
        ins = e.h.dma_start(out=out_ap, in_=in_ap)
        cnt = self.semcnt.get(sbb.dsem, 0) + 16
        self.semcnt[sbb.dsem] = cnt
        ins.then_inc(sbb.dsem, 16)
        self._commit([in_buf], [out_buf], sbb.dsem, cnt)
        return ins

    def barrier(self):
        deps = dict(self.semcnt)
        for e in self.engs.values():
            if e.count:
                deps[e.sem] = e.count
        for e in self.engs.values():
            d = dict(deps)
            self._wait(e, d)


class Defer:
    def __init__(self):
        self.p = None

    def push(self, fn):
        old, self.p = self.p, fn
        if old:
            old()

    def flush(self):
        if self.p:
            self.p()
            self.p = None


class Ring:
    def __init__(self, bufs):
        self.bufs, self.i = bufs, -1

    def next(self):
        self.i = (self.i + 1) % len(self.bufs)
        return self.bufs[self.i]


def bc_mid(ap2d, n):
    p, f = ap2d.shape
    return ap2d.unsqueeze(1).to_broadcast([p, n, f])


def bc_last(ap2d, n):
    p, f = ap2d.shape
    return ap2d.unsqueeze(2).to_broadcast([p, f, n])


def rstd_from_ss(fw, ss, n):
    fw.op("act", [ss], [ss], lambda h: h.activation(out=ss[:, 0:1], in_=ss[:, 0:1], func=ACT.Sqrt,
                                                    scale=1.0 / n, bias=EPS))
    fw.op("dve", [ss], [ss], lambda h: h.reciprocal(out=ss[:, 0:1], in_=ss[:, 0:1]))


def rms(fw, srcb, src, n, gb, g, dstb, dst, ss, junk):
    fw.op("act", [srcb], [junk, ss], lambda h: h.activation(out=junk[:, 0:n], in_=src, func=ACT.Square,
                                                            accum_out=ss[:, 0:1]))
    rstd_from_ss(fw, ss, n)
    fw.op("dve", [srcb, ss, gb], [dstb], lambda h: h.scalar_tensor_tensor(
        out=dst, in0=src, scalar=ss[:, 0:1], in1=g, op0=ALU.mult, op1=ALU.mult))


def transposes(fw, srcb, src_aps, psb, ps_aps, ident, identb):
    for s_ap, p_ap in zip(src_aps, ps_aps):
        fw.op("pe", [srcb, identb], [psb], lambda h, s_ap=s_ap, p_ap=p_ap: h.transpose(
            out=p_ap, in_=s_ap, identity=ident))


def load_bcast(fw, dst, dram_buf, row_ap, n):
    fw.dma(dst, dst[:, 0:n], dram_buf, row_ap.partition_broadcast(128))


def rope_emit(fw, srcb, src3, dstb, dst3, tabb, cos2, sin2, nh, half, tmpb, tmp3a, tmp3b):
    cb, sb_ = bc_mid(cos2, nh), bc_mid(sin2, nh)
    x1, x2 = src3[:, :, 0:half], src3[:, :, half:2 * half]
    d1, d2 = dst3[:, :, 0:half], dst3[:, :, half:2 * half]
    fw.op("dve", [srcb, tabb], [dstb], lambda h: h.tensor_tensor(out=d1, in0=x1, in1=cb, op=ALU.mult))
    fw.op("dve", [srcb, tabb], [tmpb], lambda h: h.tensor_tensor(out=tmp3a, in0=x2, in1=sb_, op=ALU.mult))
    fw.op("pool", [dstb, tmpb], [dstb], lambda h: h.tensor_tensor(out=d1, in0=d1, in1=tmp3a, op=ALU.subtract))
    fw.op("dve", [srcb, tabb], [dstb], lambda h: h.tensor_tensor(out=d2, in0=x2, in1=cb, op=ALU.mult))
    fw.op("dve", [srcb, tabb], [tmpb], lambda h: h.tensor_tensor(out=tmp3b, in0=x1, in1=sb_, op=ALU.mult))
    fw.op("pool", [dstb, tmpb], [dstb], lambda h: h.tensor_tensor(out=d2, in0=d2, in1=tmp3b, op=ALU.add))


def phase_A0(fw, X, W, G, C):
    fw.begin()
    w = fw.sb("w", [128, 8, 3072], dma=True)
    for k in range(8):
        fw.dma(w, w[:, k, :], W["w_in_ab"], W["w_in_ab"].ap[k * 128:(k + 1) * 128, :])
    g = fw.sb("g", [128, D], dma=True)
    load_bcast(fw, g, W["g_mix_pre"], W["g_mix_pre"].ap[0:1, :], D)
    ident = fw.sb("ident", [128, 128], dma=True)
    fw.dma(ident, ident[:, :], C["ident"], C["ident"].ap)
    xts = Ring([fw.sb("xt%d" % i, [128, D], dma=True) for i in range(2)])
    tabs = Ring([fw.sb("tab%d" % i, [128, 64], dma=True) for i in range(2)])
    hn = fw.sb("hn", [128, D])
    hnT = fw.sb("hnT", [128, 8, 128])
    junk = fw.sb("junk", [128, D])
    ss = fw.sb("ss", [128, 1])
    qk = fw.sb("qk", [128, 4, 8, 64])
    tmp = fw.sb("tmp", [128, 2, 8, 32])
    vts = Ring([fw.sb("vt%d" % i, [128, D], dma=True) for i in range(2)])
    qkTs = Ring([fw.sb("qkT%d" % i, [128, 16, 128], dma=True) for i in range(2)])
    pT = fw.psum("pT", [128, 1024])
    pm = Ring([fw.psum("pm%d" % i, [128, 512]) for i in range(2)])
    pq = fw.psum("pq", [128, 2048])
    for t in range(min(NT, LIM_T)):
        r = slice(t * 128, (t + 1) * 128)
        xt = xts.next()
        tab = tabs.next()
        fw.dma(xt, xt[:, :], X, X.ap[r, :])
        fw.dma(tab, tab[:, :], C["tab64"], C["tab64"].ap[r, :])
        rms(fw, xt, xt[:, :], D, g, g[:, :], hn, hn[:, :], ss, junk)
        transposes(fw, hn, [hn[:, k * 128:(k + 1) * 128] for k in range(8)], pT,
                   [pT[:, k * 128:(k + 1) * 128] for k in range(8)], ident[:, :], ident)
        fw.op("act", [pT], [hnT], lambda h: h.copy(out=hnT[:, :, :], in_=pT[:, :].rearrange("p (k t) -> p k t", k=8)))
        vt = vts.next()
        qi = 0
        for c in range(6):
            p = pm.next()
            for k in range(8):
                fw.op("pe", [hnT, w], [p], lambda h, k=k, c=c, p=p: h.matmul(
                    out=p[:, :], lhsT=hnT[:, k, :], rhs=w[:, k, c * 512:(c + 1) * 512], start=(k == 0), stop=(k == 7)))
            if c in (2, 5):
                vo = 0 if c == 2 else 512
                fw.op("act", [p], [vt], lambda h, p=p, vo=vo: h.copy(out=vt[:, vo:vo + 512], in_=p[:, :]))
            else:
                src3 = p[:, :].rearrange("p (h d) -> p h d", h=8)
                rope_emit(fw, p, src3, qk, qk[:, qi, :, :], tab, tab[:, 0:32], tab[:, 32:64], 8, 32,
                          tmp, tmp[:, 0, :, :], tmp[:, 1, :, :])
                qi += 1
        fw.dma(W["v0"], W["v0"].ap[r, :], vt, vt[:, :])
        qkf = qk[:, :, :, :].rearrange("p a h d -> p (a h d)")
        transposes(fw, qk, [qkf[:, j * 128:(j + 1) * 128] for j in range(16)], pq,
                   [pq[:, j * 128:(j + 1) * 128] for j in range(16)], ident[:, :], ident)
        qkT = qkTs.next()
        fw.op("act", [pq], [qkT], lambda h, qkT=qkT: h.copy(out=qkT[:, 0:8, :], in_=pq[:, 0:1024].rearrange("p (k t) -> p k t", k=8)))
        fw.op("dve", [pq], [qkT], lambda h, qkT=qkT: h.tensor_copy(out=qkT[:, 8:16, :], in_=pq[:, 1024:2048].rearrange("p (k t) -> p k t", k=8)))
        fw.dma(W["qkT0"], W["qkT0"].ap[:, :, r].rearrange("c p s -> p c s"), qkT, qkT[:, :, :])
    fw.end()


def attn_epilogue_store(fw, dst_dram, dst_ap, ot):
    fw.dma(dst_dram, dst_ap, ot, ot[:, :])


def phase_B0(fw, W, C):
    fw.begin()
    cm = fw.sb("cm", [128, 128], dma=True)
    fw.dma(cm, cm[:, :], C["cmaskT"], C["cmaskT"].ap)
    lv = fw.sb("lv", [128, 4, 64], dma=True)
    for i, nm in enumerate(("diff_lq1", "diff_lk1", "diff_lq2", "diff_lk2")):
        fw.dma(lv, lv[:, i, :], W[nm], W[nm].ap[0:1, :].partition_broadcast(128))
    gs = fw.sb("gs", [128, 128], dma=True)
    load_bcast(fw, gs, W["g_diff_sub"], W["g_diff_sub"].ap[0:1, :], 128)
    lt = fw.sb("lt", [128, 2, 64])
    ls = fw.sb("ls", [128, 4])
    fw.op("dve", [lv], [lt], lambda h: h.tensor_tensor(out=lt[:, 0, :], in0=lv[:, 0, :], in1=lv[:, 1, :], op=ALU.mult))
    fw.op("dve", [lv], [lt], lambda h: h.tensor_tensor(out=lt[:, 1, :], in0=lv[:, 2, :], in1=lv[:, 3, :], op=ALU.mult))
    fw.op("dve", [lt], [ls], lambda h: h.reduce_sum(out=ls[:, 0:2], in_=lt[:, :, :], axis=mybir.AxisListType.X))
    fw.op("act", [ls], [ls], lambda h: h.activation(out=ls[:, 0:2], in_=ls[:, 0:2], func=ACT.Exp))
    fw.op("dve", [ls], [ls], lambda h: h.tensor_tensor(out=ls[:, 2:3], in0=ls[:, 1:2], in1=ls[:, 0:1], op=ALU.subtract))
    fw.op("dve", [ls], [ls], lambda h: h.tensor_scalar(out=ls[:, 2:3], in0=ls[:, 2:3], scalar1=-0.2, scalar2=None, op0=ALU.add))
    fw.op("dve", [gs], [gs], lambda h: h.tensor_scalar(out=gs[:, :], in0=gs[:, :], scalar1=0.8, scalar2=None, op0=ALU.mult))
    qTs = Ring([fw.sb("qT%d" % i, [128, S], dma=True) for i in range(2)])
    kTs = Ring([fw.sb("kT%d" % i, [128, 2, S], dma=True) for i in range(2)])
    for kT in kTs.bufs:
        fw.op("pool", [], [kT], lambda h, kT=kT: h.memset(kT[64:128, 0, :], 0.0))
        fw.op("pool", [], [kT], lambda h, kT=kT: h.memset(kT[0:64, 1, :], 0.0))
    vas = Ring([fw.sb("va%d" % i, [128, NT, 130], dma=True) for i in range(2)])
    for va in vas.bufs:
        fw.op("pool", [], [va], lambda h, va=va: h.memset(va[:, :, :], 1.0))
    Ps = Ring([fw.sb("P%d" % i, [128, 256]) for i in range(3)])
    pss = Ring([fw.psum("ps%d" % i, [128, 512]) for i in range(3)])
    accs = Ring([fw.psum("acc%d" % i, [128, 512]) for i in range(2)])
    rr = fw.sb("rr", [128, 4])
    o1 = fw.sb("o1", [128, 128])
    junk = fw.sb("junk", [128, 128])
    ss = fw.sb("ss", [128, 1])
    ots = Ring([fw.sb("ot%d" % i, [128, 128], dma=True) for i in range(2)])
    df = Defer()
    for hd in range(min(4, LIM_H) if B0_STOP >= 2 else 0):
        qT, kT, va = qTs.next(), kTs.next(), vas.next()
        fw.dma(qT, qT[:, :], W["qkT0"], W["qkT0"].ap[hd, :, :])
        fw.dma(kT, kT[0:64, 0, :], W["qkT0"], W["qkT0"].ap[4 + hd, 0:64, :])
        fw.dma(kT, kT[64:128, 1, :], W["qkT0"], W["qkT0"].ap[4 + hd, 64:128, :])
        for n0 in range(0, NT, 8):
            fw.dma(va, va[:, n0:n0 + 8, 0:128], W["v0"], W["v0"].ap[n0 * 128:(n0 + 8) * 128, hd * 128:(hd + 1) * 128].rearrange("(n p) d -> p n d", p=128))
        for qi in range(LIM_Q):
            acc = accs.next()
            qs = slice(qi * 128, (qi + 1) * 128)
            for kb in range(qi + 1):
                ks = slice(kb * 128, (kb + 1) * 128)
                ps, P = pss.next(), Ps.next()
                for m in range(B0_M):
                    fw.op("pe", [qT, kT], [ps], lambda h, m=m, ps=ps, ks=ks: h.matmul(
                        out=ps[:, m * 128:(m + 1) * 128], lhsT=kT[:, m, ks], rhs=qT[:, qs], start=True, stop=True))
                fw.op("act", [ps], [P], lambda h, ps=ps, P=P: h.activation(out=P[:, :], in_=ps[:, 0:256], func=ACT.Exp, scale=0.125))
                if kb == qi:
                    fw.op("dve", [P, cm], [P], lambda h, P=P: h.tensor_tensor(
                        out=P[:, :].rearrange("p (m q) -> p m q", m=2), in0=P[:, :].rearrange("p (m q) -> p m q", m=2),
                        in1=bc_mid(cm[:, :], 2), op=ALU.mult))
                def pv(P=P, kb=kb, acc=acc, va=va, qi=qi):
                    for m in range(2 if B0_STOP >= 3 else 0):
                        fw.op("pe", [P, va], [acc], lambda h, m=m: h.matmul(
                            out=acc[:, m * 130:(m + 1) * 130], lhsT=P[:, m * 128:(m + 1) * 128], rhs=va[:, kb, :],
                            start=(kb == 0 and m == 0), stop=(kb == qi), skip_group_check=True))
                df.push(pv)
            df.flush()
            if B0_STOP < 4:
                continue
            a3 = acc[:, 0:260].rearrange("p (m d) -> p m d", m=2)
            fw.op("dve", [acc], [rr], lambda h, a3=a3: h.reciprocal(out=rr[:, 0:2], in_=a3[:, :, 128]))
            fw.op("dve", [rr, ls], [rr], lambda h: h.tensor_tensor(out=rr[:, 2:3], in0=rr[:, 1:2], in1=ls[:, 2:3], op=ALU.mult))
            fw.op("dve", [acc, rr], [o1], lambda h, a3=a3: h.tensor_scalar(out=o1[:, :], in0=a3[:, 0, 0:128], scalar1=rr[:, 0:1], scalar2=None, op0=ALU.mult))
            fw.op("dve", [acc, rr, o1], [o1], lambda h, a3=a3: h.scalar_tensor_tensor(
                out=o1[:, :], in0=a3[:, 1, 0:128], scalar=rr[:, 2:3], in1=o1[:, :], op0=ALU.mult, op1=ALU.add))
            ot = ots.next()
            rms(fw, o1, o1[:, :], 128, gs, gs[:, :], ot, ot[:, :], ss, junk)
            fw.dma(W["attn0"], W["attn0"].ap[qs, hd * 128:(hd + 1) * 128], ot, ot[:, :])
    fw.end()


def phase_C0(fw, W, C):
    fw.begin()
    dm = fw.sb("dm", [128, 17, 128], dma=True)
    for r0, r1 in ((0, 8), (8, 17)):
        fw.dma(dm, dm[:, r0:r1, :], C["dmaskT"], C["dmaskT"].ap[r0:r1].rearrange("r k q -> k r q"))
    qTs = Ring([fw.sb("qT%d" % i, [128, 2, S], dma=True) for i in range(1)])
    kTs = Ring([fw.sb("kT%d" % i, [128, 4, S], dma=True) for i in range(1)])
    for kT in kTs.bufs:
        for j in range(4):
            zr = slice(64, 128) if j % 2 == 0 else slice(0, 64)
            fw.op("pool", [], [kT], lambda h, kT=kT, j=j, zr=zr: h.memset(kT[zr, j, :], 0.0))
    vas = Ring([fw.sb("va%d" % i, [128, NT, 4, 66], dma=True) for i in range(1)])
    for va in vas.bufs:
        fw.op("pool", [], [va], lambda h, va=va: h.memset(va[:, :, :, :], 1.0))
    Ps = Ring([fw.sb("P%d" % i, [128, 512]) for i in range(3)])
    pss = Ring([fw.psum("ps%d" % i, [128, 512]) for i in range(3)])
    accs = Ring([fw.psum("acc%d" % i, [128, 512]) for i in range(2)])
    rr = fw.sb("rr", [128, 4])
    ots = Ring([fw.sb("ot%d" % i, [128, 4, 64], dma=True) for i in range(2)])
    flip = 0
    df = Defer()
    for g in range(min(2, LIM_H)):
        qT, kT, va = qTs.next(), kTs.next(), vas.next()
        for j in range(2):
            fw.dma(qT, qT[:, j, :], W["qkT0"], W["qkT0"].ap[8 + 2 * g + j, :, :])
            fw.dma(kT, kT[0:64, 2 * j, :], W["qkT0"], W["qkT0"].ap[12 + 2 * g + j, 0:64, :])
            fw.dma(kT, kT[64:128, 2 * j + 1, :], W["qkT0"], W["qkT0"].ap[12 + 2 * g + j, 64:128, :])
        for j in range(4):
            c0 = 512 + (4 * g + j) * 64
            for n0 in range(0, NT, 8):
                fw.dma(va, va[:, n0:n0 + 8, j, 0:64], W["v0"], W["v0"].ap[n0 * 128:(n0 + 8) * 128, c0:c0 + 64].rearrange("(n p) d -> p n d", p=128))
        for qi in range(LIM_Q):
            acc = accs.next()
            qs = slice(qi * 128, (qi + 1) * 128)
            kb0 = max(0, qi - 16)
            for kb in range(kb0, qi + 1):
                ks = slice(kb * 128, (kb + 1) * 128)
                ps, P = pss.next(), Ps.next()
                for j in range(4):
                    fw.op("pe", [qT, kT], [ps], lambda h, j=j, ps=ps, ks=ks: h.matmul(
                        out=ps[:, j * 128:(j + 1) * 128], lhsT=kT[:, j, ks], rhs=qT[:, j // 2, qs], start=True, stop=True))
                fw.op("act", [ps], [P], lambda h, ps=ps, P=P: h.activation(out=P[:, :], in_=ps[:, :], func=ACT.Exp, scale=0.125))
                eng = "dve" if flip else "pool"
                flip ^= 1
                fw.op(eng, [P, dm], [P], lambda h, P=P, kb=kb: h.tensor_tensor(
                    out=P[:, :].rearrange("p (m q) -> p m q", m=4), in0=P[:, :].rearrange("p (m q) -> p m q", m=4),
                    in1=bc_mid(dm[:, qi - kb, :], 4), op=ALU.mult))
                def pv(P=P, kb=kb, acc=acc, va=va, qi=qi, kb0=kb0):
                    for j in range(4):
                        fw.op("pe", [P, va], [acc], lambda h, j=j: h.matmul(
                            out=acc[:, j * 66:(j + 1) * 66], lhsT=P[:, j * 128:(j + 1) * 128], rhs=va[:, kb, j, :],
                            start=(kb == kb0 and j == 0), stop=(kb == qi), skip_group_check=True))
                df.push(pv)
            df.flush()
            a3 = acc[:, 0:264].rearrange("p (m d) -> p m d", m=4)
            fw.op("dve", [acc], [rr], lambda h, a3=a3: h.reciprocal(out=rr[:, 0:4], in_=a3[:, :, 64]))
            ot = ots.next()
            fw.op("dve", [acc, rr], [ot], lambda h, a3=a3, ot=ot: h.tensor_tensor(
                out=ot[:, :, :], in0=a3[:, :, 0:64], in1=bc_last(rr[:, 0:4], 64), op=ALU.mult))
            c0 = 512 + g * 256
            fw.dma(W["attn0"], W["attn0"].ap[qs, c0:c0 + 256], ot, ot[:, :, :].rearrange("p m d -> p (m d)"))
    fw.end()


def phase_D(fw, W, C, attn, wname, li, hin, hout):
    fw.begin()
    w = fw.sb("w", [128, 8, D], dma=True)
    fw.dma(w, w[:, :, :], W[wname], W[wname].ap.rearrange("(k p) n -> p k n", p=128))
    g = fw.sb("g", [128, D], dma=True)
    load_bcast(fw, g, W["g_mix_post"], W["g_mix_post"].ap[li:li + 1, :], D)
    ident = fw.sb("ident", [128, 128], dma=True)
    fw.dma(ident, ident[:, :], C["ident"], C["ident"].ap)
    ats = Ring([fw.sb("at%d" % i, [128, D], dma=True) for i in range(2)])
    hts = Ring([fw.sb("ht%d" % i, [128, D], dma=True) for i in range(2)])
    aT = fw.sb("aT", [128, 8, 128])
    yn = fw.sb("yn", [128, D])
    junk = fw.sb("junk", [128, D])
    ss = fw.sb("ss", [128, 1])
    pT = fw.psum("pT", [128, 1024])
    py = fw.psum("py", [128, 1024])
    for t in range(min(NT, LIM_T)):
        r = slice(t * 128, (t + 1) * 128)
        at, ht = ats.next(), hts.next()
        fw.dma(at, at[:, :], attn, attn.ap[r, :])
        fw.dma(ht, ht[:, :], hin, hin.ap[r, :])
        transposes(fw, at, [at[:, k * 128:(k + 1) * 128] for k in range(8)], pT,
                   [pT[:, k * 128:(k + 1) * 128] for k in range(8)], ident[:, :], ident)
        fw.op("act", [pT], [aT], lambda h: h.copy(out=aT[:, :, :], in_=pT[:, :].rearrange("p (k t) -> p k t", k=8)))
        for c in range(2):
            for k in range(8):
                fw.op("pe", [aT, w], [py], lambda h, k=k, c=c: h.matmul(
                    out=py[:, c * 512:(c + 1) * 512], lhsT=aT[:, k, :], rhs=w[:, k, c * 512:(c + 1) * 512],
                    start=(k == 0), stop=(k == 7)))
        rms(fw, py, py[:, :], D, g, g[:, :], yn, yn[:, :], ss, junk)
        fw.op("pool", [ht, yn], [ht], lambda h, ht=ht: h.tensor_tensor(out=ht[:, :], in0=ht[:, :], in1=yn[:, :], op=ALU.add))
        fw.dma(hout, hout.ap[r, :], ht, ht[:, :])
    fw.end()


def phase_E(fw, W, C, li, hin, hout):
    fw.begin()
    win = fw.sb("win", [128, 8, 4096], BF16)
    wout = fw.sb("wout", [128, 32, D], BF16)
    stg = Ring([fw.sb("stg%d" % i, [128, 2048], dma=True) for i in range(2)])
    n = 0
    for k in range(8):
        for c in range(2):
            s = stg.next()
            fw.dma(s, s[:, :], W["w_mlp_in"], W["w_mlp_in"].ap[li, k * 128:(k + 1) * 128, c * 2048:(c + 1) * 2048])
            fw.op(("dve", "act")[n % 2], [s], [win], (lambda h, s=s, k=k, c=c: h.tensor_copy(out=win[:, k, c * 2048:(c + 1) * 2048], in_=s[:, :])) if n % 2 == 0 else (lambda h, s=s, k=k, c=c: h.copy(out=win[:, k, c * 2048:(c + 1) * 2048], in_=s[:, :])))
            n += 1
    for k2 in range(16):
        s = stg.next()
        fw.dma(s, s[:, :].rearrange("p (a n) -> p a n", a=2), W["w_mlp_out"],
               W["w_mlp_out"].ap[li, k2 * 256:(k2 + 1) * 256, :].rearrange("(a p) n -> p a n", p=128))
        fw.op(("dve", "act")[n % 2], [s], [wout], (lambda h, s=s, k2=k2: h.tensor_copy(
            out=wout[:, 2 * k2:2 * k2 + 2, :], in_=s[:, :].rearrange("p (a n) -> p a n", a=2))) if n % 2 == 0 else (lambda h, s=s, k2=k2: h.copy(
            out=wout[:, 2 * k2:2 * k2 + 2, :], in_=s[:, :].rearrange("p (a n) -> p a n", a=2))))
        n += 1
    gpre = fw.sb("gpre", [128, D], dma=True)
    load_bcast(fw, gpre, W["g_mlp_pre"], W["g_mlp_pre"].ap[li:li + 1, :], D)
    gpost = fw.sb("gpost", [128, D], dma=True)
    load_bcast(fw, gpost, W["g_mlp_post"], W["g_mlp_post"].ap[li:li + 1, :], D)
    ident = fw.sb("ident", [128, 128], dma=True)
    fw.dma(ident, ident[:, :], C["ident"], C["ident"].ap)
    hts = Ring([fw.sb("ht%d" % i, [128, D], dma=True) for i in range(2)])
    hn = fw.sb("hn", [128, D])
    hnT = fw.sb("hnT", [128, 8, 128], BF16)
    hid = fw.sb("hid", [128, 32, 128], BF16)
    rl = Ring([fw.sb("rl%d" % i, [128, 512]) for i in range(2)])
    yn = fw.sb("yn", [128, D])
    junk = fw.sb("junk", [128, D])
    ss = fw.sb("ss", [128, 1])
    pT = fw.psum("pT", [128, 1024])
    pus = Ring([fw.psum("pu%d" % i, [128, 512]) for i in range(3)])
    py = fw.psum("py", [128, 1024])
    for t in range(min(NT, LIM_T)):
        r = slice(t * 128, (t + 1) * 128)
        ht = hts.next()
        fw.dma(ht, ht[:, :], hin, hin.ap[r, :])
        rms(fw, ht, ht[:, :], D, gpre, gpre[:, :], hn, hn[:, :], ss, junk)
        transposes(fw, hn, [hn[:, k * 128:(k + 1) * 128] for k in range(8)], pT,
                   [pT[:, k * 128:(k + 1) * 128] for k in range(8)], ident[:, :], ident)
        fw.op("act", [pT], [hnT], lambda h: h.copy(out=hnT[:, :, :], in_=pT[:, :].rearrange("p (k t) -> p k t", k=8)))
        for f4 in range(8):
            pu = pus.next()
            for f in range(4):
                fc = f4 * 4 + f
                for k in range(8):
                    fw.op("pe", [hnT, win], [pu], lambda h, k=k, f=f, fc=fc, pu=pu: h.matmul(
                        out=pu[:, f * 128:(f + 1) * 128], lhsT=win[:, k, fc * 128:(fc + 1) * 128], rhs=hnT[:, k, :],
                        start=(k == 0), stop=(k == 7)))
            rt = rl.next()
            fw.op("act", [pu], [rt], lambda h, pu=pu, rt=rt: h.activation(out=rt[:, :], in_=pu[:, :], func=ACT.Relu))
            fw.op("dve", [rt], [hid], lambda h, rt=rt, f4=f4: h.tensor_tensor(
                out=hid[:, f4 * 4:(f4 + 1) * 4, :].rearrange("p a t -> p (a t)"), in0=rt[:, :], in1=rt[:, :], op=ALU.mult))
        for c in range(2):
            for k in range(32):
                fw.op("pe", [hid, wout], [py], lambda h, k=k, c=c: h.matmul(
                    out=py[:, c * 512:(c + 1) * 512], lhsT=hid[:, k, :], rhs=wout[:, k, c * 512:(c + 1) * 512],
                    start=(k == 0), stop=(k == 31)))
        rms(fw, py, py[:, :], D, gpost, gpost[:, :], yn, yn[:, :], ss, junk)
        fw.op("pool", [ht, yn], [ht], lambda h, ht=ht: h.tensor_tensor(out=ht[:, :], in0=ht[:, :], in1=yn[:, :], op=ALU.add))
        fw.dma(hout, hout.ap[r, :], ht, ht[:, :])
    fw.end()


def phase_F(fw, W, C, P_, li, hin, hout):
    fw.begin()
    wg = fw.sb("wg", [128, 8, D], dma=True)
    fw.dma(wg, wg[:, :, :], W["w_ple_gate"], W["w_ple_gate"].ap[li].rearrange("(k p) n -> p k n", p=128))
    wp = fw.sb("wp", [128, 2, D], dma=True)
    fw.dma(wp, wp[:, :, :], W["w_ple_proj"], W["w_ple_proj"].ap[li].rearrange("(k p) n -> p k n", p=128))
    ident = fw.sb("ident", [128, 128], dma=True)
    fw.dma(ident, ident[:, :], C["ident"], C["ident"].ap)
    hts = Ring([fw.sb("ht%d" % i, [128, D], dma=True) for i in range(2)])
    pts = Ring([fw.sb("pt%d" % i, [128, 256], dma=True) for i in range(2)])
    hT = fw.sb("hT", [128, 10, 128])
    gate = fw.sb("gate", [128, D])
    pT = fw.psum("pT", [128, 1536])
    pg = fw.psum("pg", [128, 1024])
    pp = fw.psum("pp", [128, 1024])
    for t in range(min(NT, LIM_T)):
        r = slice(t * 128, (t + 1) * 128)
        ht, pt = hts.next(), pts.next()
        fw.dma(ht, ht[:, :], hin, hin.ap[r, :])
        fw.dma(pt, pt[:, :], P_, P_.ap[li, r, :])
        transposes(fw, ht, [ht[:, k * 128:(k + 1) * 128] for k in range(8)], pT,
                   [pT[:, k * 128:(k + 1) * 128] for k in range(8)], ident[:, :], ident)
        transposes(fw, pt, [pt[:, k * 128:(k + 1) * 128] for k in range(2)], pT,
                   [pT[:, (8 + k) * 128:(9 + k) * 128] for k in range(2)], ident[:, :], ident)
        fw.op("act", [pT], [hT], lambda h: h.copy(out=hT[:, :, :], in_=pT[:, 0:1280].rearrange("p (k t) -> p k t", k=10)))
        for c in range(2):
            for k in range(8):
                fw.op("pe", [hT, wg], [pg], lambda h, k=k, c=c: h.matmul(
                    out=pg[:, c * 512:(c + 1) * 512], lhsT=hT[:, k, :], rhs=wg[:, k, c * 512:(c + 1) * 512],
                    start=(k == 0), stop=(k == 7)))
        for c in range(2):
            for k in range(2):
                fw.op("pe", [hT, wp], [pp], lambda h, k=k, c=c: h.matmul(
                    out=pp[:, c * 512:(c + 1) * 512], lhsT=hT[:, 8 + k, :], rhs=wp[:, k, c * 512:(c + 1) * 512],
                    start=(k == 0), stop=(k == 1)))
        fw.op("act", [pg], [gate], lambda h: h.activation(out=gate[:, :], in_=pg[:, :], func=ACT.Sigmoid))
        fw.op("dve", [pp, gate], [gate], lambda h: h.tensor_tensor(out=gate[:, :], in0=pp[:, :], in1=gate[:, :], op=ALU.mult))
        fw.op("pool", [ht, gate], [ht], lambda h, ht=ht: h.tensor_tensor(out=ht[:, :], in0=ht[:, :], in1=gate[:, :], op=ALU.add))
        fw.dma(hout, hout.ap[r, :], ht, ht[:, :])
    fw.end()


def phase_A1(fw, W, C, hin):
    fw.begin()
    wc = fw.sb("wc", [128, 8, 744], dma=True)
    fw.dma(wc, wc[:, :, :], W["w_in_c"], W["w_in_c"].ap.rearrange("(k p) n -> p k n", p=128))
    wuq = fw.sb("wuq", [128, 3, 1536], dma=True)
    fw.dma(wuq, wuq[:, :, :], W["w_uq"], W["w_uq"].ap.rearrange("(k p) n -> p k n", p=128))
    wqi = fw.sb("wqi", [128, 3, 512], dma=True)
    fw.dma(wqi, wqi[:, :, :], W["w_qi"], W["w_qi"].ap.rearrange("(k p) n -> p k n", p=128))
    wuk = fw.sb("wuk", [128, 2, 1024], dma=True)
    fw.dma(wuk, wuk[:, :, :], W["w_uk"], W["w_uk"].ap.rearrange("(k p) n -> p k n", p=128))
    wuv = fw.sb("wuv", [128, 2, 1024], dma=True)
    fw.dma(wuv, wuv[:, :, :], W["w_uv"], W["w_uv"].ap.rearrange("(k p) n -> p k n", p=128))
    g = fw.sb("g", [128, D], dma=True)
    load_bcast(fw, g, W["g_mix_pre"], W["g_mix_pre"].ap[1:2, :], D)
    gq = fw.sb("gq", [128, 384], dma=True)
    load_bcast(fw, gq, W["g_cq"], W["g_cq"].ap[0:1, :], 384)
    gkv = fw.sb("gkv", [128, 256], dma=True)
    load_bcast(fw, gkv, W["g_ckv"], W["g_ckv"].ap[0:1, :], 256)
    ident = fw.sb("ident", [128, 128], dma=True)
    fw.dma(ident, ident[:, :], C["ident"], C["ident"].ap)
    pA = fw.psum("pA", [128, 2048])
    pB = fw.psum("pB", [128, 2048])
    wukT = fw.sb("wukT", [64, 16, 256])
    for cc in range(2):
        transposes(fw, wuk, [wuk[:, cc, hh * 64:(hh + 1) * 64] for hh in range(16)], pA,
                   [pA[0:64, hh * 128:(hh + 1) * 128] for hh in range(16)], ident[:, :], ident)
        fw.op("act", [pA], [wukT], lambda h, cc=cc: h.copy(
            out=wukT[:, :, cc * 128:(cc + 1) * 128], in_=pA[0:64, :].rearrange("p (h c) -> p h c", h=16)))
    hts = Ring([fw.sb("ht%d" % i, [128, D], dma=True) for i in range(2)])
    tabs = Ring([fw.sb("tab%d" % i, [128, 32], dma=True) for i in range(2)])
    hn = fw.sb("hn", [128, D])
    hnT = fw.sb("hnT", [128, 8, 128])
    junk = fw.sb("junk", [128, D])
    ss = fw.sb("ss", [128, 1])
    csb = fw.sb("csb", [128, 744])
    cqn = fw.sb("cqn", [128, 384])
    cqT = fw.sb("cqT", [128, 3, 128])
    kvtok = fw.sb("kvtok", [128, 288])
    kis = fw.sb("kis", [128, 64])
    tmp = fw.sb("tmp", [128, 2, 16, 16])
    wis = Ring([fw.sb("wis%d" % i, [128, 8], dma=True) for i in range(2)])
    qn = fw.sb("qn", [128, 16, 64])
    qsb = fw.sb("qsb", [128, 1536])
    qr = fw.sb("qr", [128, 16, 32])
    qnT = fw.sb("qnT", [64, 16, 128])
    qfTs = Ring([fw.sb("qfT%d" % i, [128, 3, 2048], dma=True) for i in range(1)])
    kvTs = Ring([fw.sb("kvT%d" % i, [128, 3, 128], dma=True) for i in range(2)])
    vhs = Ring([fw.sb("vh%d" % i, [128, D], dma=True) for i in range(2)])
    qis = fw.sb("qis", [128, 8, 64])
    qiTs = Ring([fw.sb("qiT%d" % i, [64, 8, 128], dma=True) for i in range(2)])
    kiTs = Ring([fw.sb("kiT%d" % i, [64, 128], dma=True) for i in range(2)])
    for t in range(min(NT, LIM_T) if A1_STOP >= 2 else 0):
        r = slice(t * 128, (t + 1) * 128)
        ht, tab = hts.next(), tabs.next()
        fw.dma(ht, ht[:, :], hin, hin.ap[r, :])
        fw.dma(tab, tab[:, :], C["tab32"], C["tab32"].ap[r, :])
        cos, sin = tab[:, 0:16], tab[:, 16:32]
        rms(fw, ht, ht[:, :], D, g, g[:, :], hn, hn[:, :], ss, junk)
        transposes(fw, hn, [hn[:, k * 128:(k + 1) * 128] for k in range(8)], pA,
                   [pA[:, k * 128:(k + 1) * 128] for k in range(8)], ident[:, :], ident)
        fw.op("act", [pA], [hnT], lambda h: h.copy(out=hnT[:, :, :], in_=pA[:, 0:1024].rearrange("p (k t) -> p k t", k=8)))
        for c, (c0, cw) in enumerate(((0, 512), (512, 232))):
            for k in range(8):
                fw.op("pe", [hnT, wc], [pB], lambda h, k=k, c=c, c0=c0, cw=cw: h.matmul(
                    out=pB[:, c * 512:c * 512 + cw], lhsT=hnT[:, k, :], rhs=wc[:, k, c0:c0 + cw], start=(k == 0), stop=(k == 7)))
        fw.op("act", [pB], [csb], lambda h: h.copy(out=csb[:, 0:512], in_=pB[:, 0:512]))
        fw.op("dve", [pB], [csb], lambda h: h.tensor_copy(out=csb[:, 512:744], in_=pB[:, 512:744]))
        rms(fw, csb, csb[:, 0:384], 384, gq, gq[:, :], cqn, cqn[:, :], ss, junk)
        rms(fw, csb, csb[:, 384:640], 256, gkv, gkv[:, :], kvtok, kvtok[:, 0:256], ss, junk)
        rope_emit(fw, csb, csb[:, 640:672].rearrange("p (h d) -> p h d", h=1), kvtok,
                  kvtok[:, 256:288].rearrange("p (h d) -> p h d", h=1), tab, cos, sin, 1, 16,
                  tmp, tmp[:, 0, 0:1, :], tmp[:, 1, 0:1, :])
        rope_emit(fw, csb, csb[:, 672:704].rearrange("p (h d) -> p h d", h=1), kis,
                  kis[:, 0:32].rearrange("p (h d) -> p h d", h=1), tab, cos, sin, 1, 16,
                  tmp, tmp[:, 0, 0:1, :], tmp[:, 1, 0:1, :])
        fw.op("act", [csb], [kis], lambda h: h.copy(out=kis[:, 32:64], in_=csb[:, 704:736]))
        wi = wis.next()
        fw.op("act", [csb], [wi], lambda h, wi=wi: h.mul(out=wi[:, :], in_=csb[:, 736:744], mul=float(8.0 ** -0.5 * 64.0 ** -0.5)))
        fw.dma(W["wi"], W["wi"].ap[r, :], wi, wi[:, :])
        if A1_STOP < 3:
            continue
        transposes(fw, cqn, [cqn[:, k * 128:(k + 1) * 128] for k in range(3)], pA,
                   [pA[:, 1024 + k * 128:1024 + (k + 1) * 128] for k in range(3)], ident[:, :], ident)
        fw.op("act", [pA], [cqT], lambda h: h.copy(out=cqT[:, :, :], in_=pA[:, 1024:1408].rearrange("p (k t) -> p k t", k=3)))
        for c in range(3):
            for k in range(3):
                fw.op("pe", [cqT, wuq], [pB], lambda h, k=k, c=c: h.matmul(
                    out=pB[:, c * 512:(c + 1) * 512], lhsT=cqT[:, k, :], rhs=wuq[:, k, c * 512:(c + 1) * 512],
                    start=(k == 0), stop=(k == 2)))
        fw.op("act", [pB], [qsb], lambda h: h.copy(out=qsb[:, 0:1024], in_=pB[:, 0:1024]))
        fw.op("dve", [pB], [qsb], lambda h: h.tensor_copy(out=qsb[:, 1024:1536], in_=pB[:, 1024:1536]))
        q3 = qsb[:, :].rearrange("p (h d) -> p h d", h=16)
        fw.op("act", [qsb], [qn], lambda h, q3=q3: h.copy(out=qn[:, :, :], in_=q3[:, :, 0:64]))
        rope_emit(fw, qsb, q3[:, :, 64:96], qr, qr[:, :, :], tab, cos, sin, 16, 16, tmp, tmp[:, 0, :, :], tmp[:, 1, :, :])
        for k in range(3):
            fw.op("pe", [cqT, wqi], [pB], lambda h, k=k: h.matmul(
                out=pB[:, 1536:2048], lhsT=cqT[:, k, :], rhs=wqi[:, k, :], start=(k == 0), stop=(k == 2)))
        qi3 = pB[:, 1536:2048].rearrange("p (h d) -> p h d", h=8)
        rope_emit(fw, pB, qi3[:, :, 0:32], qis, qis[:, :, 0:32], tab, cos, sin, 8, 16, tmp, tmp[:, 0, 0:8, :], tmp[:, 1, 0:8, :])
        fw.op("act", [pB], [qis], lambda h, qi3=qi3: h.copy(out=qis[:, :, 32:64], in_=qi3[:, :, 32:64]))
        if A1_STOP < 4:
            continue
        transposes(fw, qn, [qn[:, hh, :] for hh in range(16)], pA,
                   [pA[0:64, hh * 128:(hh + 1) * 128] for hh in range(16)], ident[:, :], ident)
        fw.op("act", [pA], [qnT], lambda h: h.copy(out=qnT[:, :, :], in_=pA[0:64, :].rearrange("p (h t) -> p h t", h=16)))
        qfT = qfTs.next()
        for cc in range(2):
            for hh in range(16):
                fw.op("pe", [qnT, wukT], [pA], lambda h, hh=hh, cc=cc: h.matmul(
                    out=pA[:, hh * 128:(hh + 1) * 128], lhsT=wukT[:, hh, cc * 128:(cc + 1) * 128], rhs=qnT[:, hh, :],
                    start=True, stop=True))
            fw.op(("act", "dve")[cc], [pA], [qfT], (lambda h, qfT=qfT: h.copy(out=qfT[:, 0, :], in_=pA[:, :])) if cc == 0 else
                  (lambda h, qfT=qfT: h.tensor_copy(out=qfT[:, 1, :], in_=pA[:, :])))
        transposes(fw, qr, [qr[:, hh, :] for hh in range(16)], pA,
                   [pA[0:32, hh * 128:(hh + 1) * 128] for hh in range(16)], ident[:, :], ident)
        fw.op("act", [pA], [qfT], lambda h, qfT=qfT: h.copy(out=qfT[0:32, 2, :], in_=pA[0:32, :]))
        fw.dma(W["qfT"], W["qfT"].ap[t, 0:256, :].rearrange("(c p) n -> p c n", p=128), qfT, qfT[:, 0:2, :])
        fw.dma(W["qfT"], W["qfT"].ap[t, 256:288, :], qfT, qfT[0:32, 2, :])
        if A1_STOP < 5:
            continue
        kvT = kvTs.next()
        transposes(fw, kvtok, [kvtok[:, 0:128], kvtok[:, 128:256]], pA,
                   [pA[:, 0:128], pA[:, 128:256]], ident[:, :], ident)
        transposes(fw, kvtok, [kvtok[:, 256:288]], pA, [pA[0:32, 256:384]], ident[:, :], ident)
        fw.op("act", [pA], [kvT], lambda h, kvT=kvT: h.copy(out=kvT[:, 0:2, :], in_=pA[:, 0:256].rearrange("p (k t) -> p k t", k=2)))
        fw.op("dve", [pA], [kvT], lambda h, kvT=kvT: h.tensor_copy(out=kvT[0:32, 2, :], in_=pA[0:32, 256:384]))
        fw.dma(W["kvT"], W["kvT"].ap[0:256, r].rearrange("(c p) s -> p c s", p=128), kvT, kvT[:, 0:2, :])
        fw.dma(W["kvT"], W["kvT"].ap[256:288, r], kvT, kvT[0:32, 2, :])
        for c in range(2):
            for k in range(2):
                fw.op("pe", [kvT, wuv], [pB], lambda h, k=k, c=c, kvT=kvT: h.matmul(
                    out=pB[:, c * 512:(c + 1) * 512], lhsT=kvT[:, k, :], rhs=wuv[:, k, c * 512:(c + 1) * 512],
                    start=(k == 0), stop=(k == 1)))
        vh = vhs.next()
        fw.op("act", [pB], [vh], lambda h, vh=vh: h.copy(out=vh[:, :], in_=pB[:, 0:1024]))
        fw.dma(W["v1"], W["v1"].ap[r, :], vh, vh[:, :])
        if A1_STOP < 6:
            continue
        transposes(fw, qis, [qis[:, hh, :] for hh in range(8)], pA,
                   [pA[0:64, 512 + hh * 128:512 + (hh + 1) * 128] for hh in range(8)], ident[:, :], ident)
        transposes(fw, kis, [kis[:, :]], pA, [pA[0:64, 1536:1664]], ident[:, :], ident)
        qiT, kiT = qiTs.next(), kiTs.next()
        fw.op("act", [pA], [qiT], lambda h, qiT=qiT: h.copy(out=qiT[:, :, :], in_=pA[0:64, 512:1536].rearrange("p (h t) -> p h t", h=8)))
        fw.op("dve", [pA], [kiT], lambda h, kiT=kiT: h.tensor_copy(out=kiT[:, :], in_=pA[0:64, 1536:1664]))
        fw.dma(W["qiT"], W["qiT"].ap[t, :, :], qiT, qiT[:, :, :].rearrange("p h t -> p (h t)"))
        fw.dma(W["kiT"], W["kiT"].ap[:, r], kiT, kiT[:, :])
    fw.end()


def phase_B1a(fw, W, C):
    fw.begin()
    ident = fw.sb("ident", [128, 128], dma=True)
    fw.dma(ident, ident[:, :], C["ident"], C["ident"].ap)
    cb = fw.sb("cb", [128, 128], dma=True)
    fw.dma(cb, cb[:, :], C["cbias"], C["cbias"].ap)
    kiT = fw.sb("kiT", [64, S], dma=True)
    fw.dma(kiT, kiT[:, :], W["kiT"], W["kiT"].ap)
    qiTs = Ring([fw.sb("qiT%d" % i, [64, 8, 128], dma=True) for i in range(2)])
    wis = Ring([fw.sb("wi%d" % i, [128, 8], dma=True) for i in range(2)])
    score = fw.sb("score", [128, S])
    work = fw.sb("work", [128, S])
    m8 = fw.sb("m8", [128, 8])
    rls = Ring([fw.sb("rl%d" % i, [128, 512]) for i in range(3)])
    mTs = Ring([fw.sb("mT%d" % i, [128, NT, 128], dma=True) for i in range(2)])
    pss = Ring([fw.psum("ps%d" % i, [128, 512]) for i in range(4)])
    pts = Ring([fw.psum("pt%d" % i, [128, 512]) for i in range(2)])
    for qi in range(LIM_Q):
        n = (qi + 1) * 128
        qiT, wi = qiTs.next(), wis.next()
        fw.dma(qiT, qiT[:, :, :].rearrange("p h t -> p (h t)"), W["qiT"], W["qiT"].ap[qi, :, :])
        fw.dma(wi, wi[:, :], W["wi"], W["wi"].ap[qi * 128:(qi + 1) * 128, :])
        for c0 in range(0, n, 512):
            cw = min(512, n - c0)
            for hh in range(8):
                ps, rl = pss.next(), rls.next()
                fw.op("pe", [qiT, kiT], [ps], lambda h, hh=hh, ps=ps, c0=c0, cw=cw, qiT=qiT: h.matmul(
                    out=ps[:, 0:cw], lhsT=qiT[:, hh, :], rhs=kiT[:, c0:c0 + cw], start=True, stop=True))
                fw.op("act", [ps], [rl], lambda h, ps=ps, rl=rl, cw=cw: h.activation(out=rl[:, 0:cw], in_=ps[:, 0:cw], func=ACT.Relu))
                if hh == 0:
                    fw.op("dve", [rl, wi], [score], lambda h, rl=rl, wi=wi, c0=c0, cw=cw: h.tensor_scalar(
                        out=score[:, c0:c0 + cw], in0=rl[:, 0:cw], scalar1=wi[:, 0:1], scalar2=None, op0=ALU.mult))
                else:
                    fw.op("dve", [rl, wi, score], [score], lambda h, rl=rl, wi=wi, c0=c0, cw=cw, hh=hh: h.scalar_tensor_tensor(
                        out=score[:, c0:c0 + cw], in0=rl[:, 0:cw], scalar=wi[:, hh:hh + 1], in1=score[:, c0:c0 + cw],
                        op0=ALU.mult, op1=ALU.add))
        fw.op("dve", [score, cb], [score], lambda h, n=n: h.tensor_tensor(
            out=score[:, n - 128:n], in0=score[:, n - 128:n], in1=cb[:, :], op=ALU.add))
        if qi < 2:
            fw.op("dve", [score], [work], lambda h, n=n: h.tensor_scalar(
                out=work[:, 0:n], in0=score[:, 0:n], scalar1=-1.0e29, scalar2=None, op0=ALU.is_ge))
        else:
            src = score
            for it in range(32):
                fw.op("dve", [src], [m8], lambda h, src=src, n=n: h.max(out=m8[:, :], in_=src[:, 0:n]))
                if it < 31:
                    fw.op("dve", [src, m8], [work], lambda h, src=src, n=n: h.match_replace(
                        out=work[:, 0:n], in_to_replace=m8[:, :], in_values=src[:, 0:n], imm_value=NEG))
                    src = work
            fw.op("dve", [score, m8], [work], lambda h, n=n: h.tensor_scalar(
                out=work[:, 0:n], in0=score[:, 0:n], scalar1=m8[:, 7:8], scalar2=None, op0=ALU.is_ge))
        mT = mTs.next()
        for k0 in range(0, qi + 1, 4):
            kn = min(4, qi + 1 - k0)
            pt = pts.next()
            transposes(fw, work, [work[:, (k0 + j) * 128:(k0 + j + 1) * 128] for j in range(kn)], pt,
                       [pt[:, j * 128:(j + 1) * 128] for j in range(kn)], ident[:, :], ident)
            fw.op("act", [pt], [mT], lambda h, pt=pt, mT=mT, k0=k0, kn=kn: h.copy(
                out=mT[:, k0:k0 + kn, :], in_=pt[:, 0:kn * 128].rearrange("p (k q) -> p k q", k=kn)))
        for k0 in range(0, qi + 1, 8):
            k1 = min(qi + 1, k0 + 8)
            fw.dma(W["maskT"], W["maskT"].ap[qi, :, k0:k1, :], mT, mT[:, k0:k1, :])
    fw.end()


def phase_B1b(fw, W, C):
    fw.begin()
    kvT = fw.sb("kvT", [128, 3, S], BF16)
    stg = Ring([fw.sb("stg%d" % i, [128, 3, 512], dma=True) for i in range(2)])
    for st in stg.bufs:
        fw.op("pool", [], [st], lambda h, st=st: h.memset(st[:, :, :], 0.0))
    for c0 in range(0, S, 512):
        st = stg.next()
        fw.dma(st, st[:, 0:2, :], W["kvT"], W["kvT"].ap[0:256, c0:c0 + 512].rearrange("(c p) s -> p c s", p=128))
        fw.dma(st, st[0:32, 2, :], W["kvT"], W["kvT"].ap[256:288, c0:c0 + 512])
        fw.op(("dve", "act")[(c0 // 512) % 2], [st], [kvT], (lambda h, st=st, c0=c0: h.tensor_copy(out=kvT[:, :, c0:c0 + 512], in_=st[:, :, :]))
              if (c0 // 512) % 2 == 0 else (lambda h, st=st, c0=c0: h.copy(out=kvT[:, :, c0:c0 + 512], in_=st[:, :, :])))
    qfs = Ring([fw.sb("qf%d" % i, [128, 3, 2048], dma=True) for i in range(1)])
    for qf in qfs.bufs:
        fw.op("pool", [], [qf], lambda h, qf=qf: h.memset(qf[:, 2, :], 0.0))
    qfTs = Ring([fw.sb("qfT%d" % i, [128, 3, 2048], BF16) for i in range(2)])
    mTs = Ring([fw.sb("mT%d" % i, [128, NT, 128], dma=True) for i in range(2)])
    vas = Ring([fw.sb("va%d" % i, [128, 16, 66], dma=True) for i in range(3)])
    for va in vas.bufs:
        fw.op("pool", [], [va], lambda h, va=va: h.memset(va[:, :, :], 1.0))
    vbs = Ring([fw.sb("vb%d" % i, [128, 16, 66], BF16) for i in range(3)])
    Ps = Ring([fw.sb("P%d" % i, [128, 512]) for i in range(4)])
    Pbs = Ring([fw.sb("Pb%d" % i, [128, 512], BF16) for i in range(4)])
    pss = Ring([fw.psum("ps%d" % i, [128, 512]) for i in range(4)])
    acc = fw.psum("acc", [128, 2048])
    rr = fw.sb("rr", [128, 16])
    ots = Ring([fw.sb("ot%d" % i, [128, 16, 64], dma=True) for i in range(2)])
    df = Defer()
    for qi in range(LIM_Q):
        qs = slice(qi * 128, (qi + 1) * 128)
        qf, qfT, mT = qfs.next(), qfTs.next(), mTs.next()
        fw.dma(qf, qf[:, 0:2, :], W["qfT"], W["qfT"].ap[qi, 0:256, :].rearrange("(c p) n -> p c n", p=128))
        fw.dma(qf, qf[0:32, 2, :], W["qfT"], W["qfT"].ap[qi, 256:288, :])
        fw.op("act", [qf], [qfT], lambda h, qf=qf, qfT=qfT: h.copy(out=qfT[:, 0:2, :], in_=qf[:, 0:2, :]))
        fw.op("dve", [qf], [qfT], lambda h, qf=qf, qfT=qfT: h.tensor_copy(out=qfT[:, 2, :], in_=qf[:, 2, :]))
        for k0 in range(0, qi + 1, 8):
            k1 = min(qi + 1, k0 + 8)
            fw.dma(mT, mT[:, k0:k1, :], W["maskT"], W["maskT"].ap[qi, :, k0:k1, :])
        for kb in range(qi + 1):
            ks = slice(kb * 128, (kb + 1) * 128)
            va, vb = vas.next(), vbs.next()
            fw.dma(va, va[:, :, 0:64], W["v1"], W["v1"].ap[ks, :].rearrange("p (h d) -> p h d", h=16))
            fw.op("act", [va], [vb], lambda h, va=va, vb=vb: h.copy(out=vb[:, :, :], in_=va[:, :, :]))
            for g in range(4):
                ps, P, Pb = pss.next(), Ps.next(), Pbs.next()
                for c in range(3):
                    rows = slice(0, 128) if c < 2 else slice(0, 32)
                    fw.op("pe", [qfT, kvT], [ps], lambda h, c=c, rows=rows, ps=ps, ks=ks, g=g, qfT=qfT: h.matmul(
                        out=ps[:, :], lhsT=kvT[rows, c, ks], rhs=qfT[rows, c, g * 512:(g + 1) * 512],
                        start=(c == 0), stop=(c == 2)))
                fw.op("act", [ps], [P], lambda h, ps=ps, P=P: h.activation(out=P[:, :], in_=ps[:, :], func=ACT.Exp, scale=float(C_SCALE)))
                fw.op("dve", [P, mT], [Pb], lambda h, P=P, Pb=Pb, kb=kb, mT=mT: h.tensor_tensor(
                    out=Pb[:, :].rearrange("p (m q) -> p m q", m=4), in0=P[:, :].rearrange("p (m q) -> p m q", m=4),
                    in1=bc_mid(mT[:, kb, :], 4), op=ALU.mult))
                def pv(Pb=Pb, vb=vb, kb=kb, qi=qi, g=g):
                    for j in range(4):
                        hh = g * 4 + j
                        bank, slot = hh // 6, hh % 6
                        o0 = bank * 512 + slot * 66
                        fw.op("pe", [Pb, vb], [acc], lambda h, j=j, hh=hh, o0=o0, slot=slot: h.matmul(
                            out=acc[:, o0:o0 + 66], lhsT=Pb[:, j * 128:(j + 1) * 128], rhs=vb[:, hh, :],
                            start=(kb == 0 and slot == 0), stop=(kb == qi), skip_group_check=True))
                df.push(pv)
        df.flush()
        ot = ots.next()
        for bank in range(3):
            nh = 6 if bank < 2 else 4
            a3 = acc[:, bank * 512:bank * 512 + nh * 66].rearrange("p (m d) -> p m d", m=nh)
            fw.op("dve", [acc], [rr], lambda h, a3=a3, bank=bank, nh=nh: h.reciprocal(out=rr[:, bank * 6:bank * 6 + nh], in_=a3[:, :, 64]))
            fw.op("dve", [acc, rr], [ot], lambda h, a3=a3, ot=ot, bank=bank, nh=nh: h.tensor_tensor(
                out=ot[:, bank * 6:bank * 6 + nh, :], in0=a3[:, :, 0:64], in1=bc_last(rr[:, bank * 6:bank * 6 + nh], 64), op=ALU.mult))
        fw.dma(W["attn1"], W["attn1"].ap[qs, :], ot, ot[:, :, :].rearrange("p m d -> p (m d)"))
    fw.end()


WEIGHT_SHAPES = {
    "g_mix_pre": [2, D], "g_mix_post": [2, D], "g_mlp_pre": [2, D], "g_mlp_post": [2, D],
    "w_mlp_in": [2, D, 4096], "w_mlp_out": [2, 4096, D], "w_ple_proj": [2, 256, D], "w_ple_gate": [2, D, D],
    "w_in_ab": [D, 3072], "w_out_ab": [D, D], "diff_lq1": [1, 64], "diff_lk1": [1, 64], "diff_lq2": [1, 64],
    "diff_lk2": [1, 64], "g_diff_sub": [1, 128], "w_in_c": [D, 744], "g_cq": [1, 384], "g_ckv": [1, 256],
    "w_uq": [384, 1536], "w_qi": [384, 512], "w_uk": [256, 1024], "w_uv": [256, 1024], "w_out_c": [D, D],
}
CONST_SHAPES = {"ident": [128, 128], "tab64": [S, 64], "tab32": [S, 32], "cmaskT": [128, 128],
                "cbias": [128, 128], "dmaskT": [17, 128, 128]}
SCRATCH_SHAPES = {
    "qkT0": [16, 128, S], "v0": [S, D], "attn0": [S, D], "h0a": [S, D], "h0b": [S, D], "h0c": [S, D],
    "qfT": [NT, 288, 2048], "kvT": [288, S], "v1": [S, D], "qiT": [NT, 64, 1024], "kiT": [64, S], "wi": [S, 8],
    "maskT": [NT, 128, NT, 128], "attn1": [S, D], "h1a": [S, D], "h1b": [S, D],
}


def build(only=None, ext_in=(), ext_out=()):
    nc = bass.Bass("TRN2", target_bir_lowering=False)
    W, C = {}, {}
    X = Buf("x", nc.dram_tensor("x", [S, D], F32, kind="ExternalInput").ap(), accumulate=True)
    P_ = Buf("p", nc.dram_tensor("p", [2, S, 256], F32, kind="ExternalInput").ap(), accumulate=True)
    for k, shp in WEIGHT_SHAPES.items():
        W[k] = Buf(k, nc.dram_tensor(k, shp, F32, kind="ExternalInput").ap(), accumulate=True)
    for k, shp in CONST_SHAPES.items():
        C[k] = Buf(k, nc.dram_tensor(k, shp, F32, kind="ExternalInput").ap(), accumulate=True)
    for k, shp in SCRATCH_SHAPES.items():
        kind = "ExternalInput" if k in ext_in else ("ExternalOutput" if k in ext_out else "Internal")
        W[k] = Buf(k, nc.dram_tensor(k, shp, F32, kind=kind).ap(), accumulate=True)
    OUT = Buf("out", nc.dram_tensor("out", [S, D], F32, kind="ExternalOutput").ap(), accumulate=True)
    phases = [
        lambda: phase_A0(fw, X, W, None, C),
        lambda: phase_B0(fw, W, C),
        lambda: phase_C0(fw, W, C),
        lambda: phase_D(fw, W, C, W["attn0"], "w_out_ab", 0, X, W["h0a"]),
        lambda: phase_E(fw, W, C, 0, W["h0a"], W["h0b"]),
        lambda: phase_F(fw, W, C, P_, 0, W["h0b"], W["h0c"]),
        lambda: phase_A1(fw, W, C, W["h0c"]),
        lambda: phase_B1a(fw, W, C),
        lambda: phase_B1b(fw, W, C),
        lambda: phase_D(fw, W, C, W["attn1"], "w_out_c", 1, W["h0c"], W["h1a"]),
        lambda: phase_E(fw, W, C, 1, W["h1a"], W["h1b"]),
        lambda: phase_F(fw, W, C, P_, 1, W["h1b"], OUT),
    ]
    with ExitStack() as es:
        fw = FW(nc, es)
        for i, ph in enumerate(phases):
            if only is not None and i not in only:
                continue
            ph()
        fw.barrier()
    return nc


def make_consts():
    c = {}
    c["ident"] = np.eye(128, dtype=np.float32)
    pos = np.arange(S, dtype=np.float32)
    for half, nm in ((32, "tab64"), (16, "tab32")):
        inv = (np.float32(10000.0) ** (-np.arange(half, dtype=np.float32) / np.float32(half))).astype(np.float32)
        ang = (pos[:, None] * inv[None, :]).astype(np.float32)
        c[nm] = np.concatenate([np.cos(ang), np.sin(ang)], axis=1).astype(np.float32)
    k = np.arange(128)[:, None]
    q = np.arange(128)[None, :]
    c["cmaskT"] = (k <= q).astype(np.float32)
    c["cbias"] = np.where(np.arange(128)[None, :] <= np.arange(128)[:, None], 0.0, NEG).astype(np.float32)
    dm = np.zeros((17, 128, 128), np.float32)
    for rel in range(17):
        dist = rel * 128 + q - k
        for w_, d_ in ((128, 1), (512, 4), (2048, 16)):
            dm[rel] += ((dist >= 0) & (dist <= w_) & (dist % d_ == 0)).astype(np.float32)
    c["dmaskT"] = dm
    return c


def make_in_maps(inputs, ncores=8):
    consts = make_consts()
    shared = {}
    for k in WEIGHT_SHAPES:
        a = np.ascontiguousarray(np.asarray(inputs[k], dtype=np.float32))
        shared[k] = a.reshape(WEIGHT_SHAPES[k])
    shared.update(consts)
    x = np.asarray(inputs["x"], dtype=np.float32)
    p = np.asarray(inputs["p"], dtype=np.float32)
    maps = []
    for b in range(ncores):
        m = dict(shared)
        m["x"] = np.ascontiguousarray(x[b])
        m["p"] = np.ascontiguousarray(p[:, b])
        maps.append(m)
    return maps


def kernel(**inputs):
    nc = build()
    maps = make_in_maps(inputs, 8)
    res = run_bass_kernel_spmd(nc, maps, core_ids=list(range(8)))
    return np.stack([np.asarray(r["out"], dtype=np.float32) for r in res.results], axis=0)
```
